# Optimizing a Trainium2 kernel written in Bass

```python
import math
import jax, jax.numpy as jnp
from jax import lax
import numpy as np

D_MODEL = 1024
BATCH = 2
SEQ = 8192
DEPTH = 1

CTX_LEN = 256
GRID_W = 64
HEAD_DIM = 64
GQA_HEADS = 8
GQA_KV_HEADS = 2
GQA_GROUP = GQA_HEADS // GQA_KV_HEADS
DIFF_HEADS = 4
DIFF_V_DIM = 2 * HEAD_DIM
MIX_WIDTH = GQA_HEADS * HEAD_DIM + DIFF_HEADS * DIFF_V_DIM
A_Q_COLS = GQA_HEADS * HEAD_DIM
A_KV_COLS = GQA_KV_HEADS * HEAD_DIM
B_QK_COLS = DIFF_HEADS * 2 * HEAD_DIM
B_V_COLS = DIFF_HEADS * DIFF_V_DIM
QKV_COLS = A_Q_COLS + 2 * A_KV_COLS + 2 * B_QK_COLS + B_V_COLS
Q_BLOCK = 128
ROPE_THETA = 10000.0
N_EXPERTS = 32
TOP_K = 4
D_FF = D_MODEL
SWIGLU_ALPHA = 1.702
SWIGLU_LIMIT = 7.0
EXPERT_BLOCK = 128
N_MOD = 6
EPS = 1e-6

kernel_name = 'hybrid_gqa_diffattn_moe_dit_layer'


def rms_norm(x, g):
    xf = x.astype(jnp.float32)
    y = xf * lax.rsqrt(jnp.mean(xf * xf, axis=-1, keepdims=True) + EPS)
    return (y * g.astype(jnp.float32)).astype(x.dtype)


def modulate(h, shift, scale):
    return h * (1 + scale) + shift


def axial_rope_tables(n_tokens):
    rows = n_tokens // GRID_W
    row = jnp.broadcast_to(jnp.arange(rows, dtype=jnp.float32)[:, None], (rows, GRID_W)).reshape(-1)
    col = jnp.broadcast_to(jnp.arange(GRID_W, dtype=jnp.float32)[None, :], (rows, GRID_W)).reshape(-1)
    n_freq = HEAD_DIM // 4
    inv_freq = ROPE_THETA ** (-jnp.arange(n_freq, dtype=jnp.float32) / n_freq)
    ang = jnp.stack([row[:, None] * inv_freq, col[:, None] * inv_freq], axis=1)
    return jnp.cos(ang), jnp.sin(ang)


def apply_axial_rope(x, cos, sin):
    b, t, h, _ = x.shape
    xr = x.reshape(b, t, h, 2, 2, HEAD_DIM // 4)
    x1, x2 = xr[..., 0, :], xr[..., 1, :]
    cs = cos[None, :, None]
    sn = sin[None, :, None]
    out = jnp.stack([x1 * cs - x2 * sn, x2 * cs + x1 * sn], axis=-2)
    return out.reshape(x.shape).astype(x.dtype)


def split_projection(p):
    b, t, _ = p.shape
    offs = [A_Q_COLS, A_Q_COLS + A_KV_COLS, A_Q_COLS + 2 * A_KV_COLS,
            A_Q_COLS + 2 * A_KV_COLS + B_QK_COLS, A_Q_COLS + 2 * A_KV_COLS + 2 * B_QK_COLS]
    qa, ka, va, qb, kb, vb = jnp.split(p, offs, axis=-1)
    qa = qa.reshape(b, t, GQA_HEADS, HEAD_DIM)
    ka = ka.reshape(b, t, GQA_KV_HEADS, HEAD_DIM)
    va = va.reshape(b, t, GQA_KV_HEADS, HEAD_DIM)
    qb = qb.reshape(b, t, DIFF_HEADS, 2, HEAD_DIM)
    kb = kb.reshape(b, t, DIFF_HEADS, 2, HEAD_DIM)
    vb = vb.reshape(b, t, DIFF_HEADS, DIFF_V_DIM)
    return qa, ka, va, qb, kb, vb


def rope_diff(x, cos, sin):
    b, t, h, two, d = x.shape
    return apply_axial_rope(x.reshape(b, t, h * two, d), cos, sin).reshape(x.shape)


def gqa_attention(q, k, v, q_block):
    b, t, _, d = q.shape
    nb = t // q_block
    qb = q.reshape(b, nb, q_block, GQA_KV_HEADS, GQA_GROUP, d).transpose(1, 0, 3, 4, 2, 5)
    scale = d ** -0.5

    def block(qi):
        s = jnp.einsum('bhgqd,bkhd->bhgqk', qi, k, preferred_element_type=jnp.float32) * scale
        p = jax.nn.softmax(s, axis=-1)
        return jnp.einsum('bhgqk,bkhd->bqhgd', p.astype(v.dtype), v)

    o = lax.map(block, qb)
    return o.transpose(1, 0, 2, 3, 4, 5).reshape(b, t, GQA_HEADS * d)


def diff_attention(q, k, v, lam, sub_g, lambda_init, q_block):
    b, t, h, _, d = q.shape
    nb = t // q_block
    qb = q.reshape(b, nb, q_block, h, 2, d).transpose(1, 0, 3, 4, 2, 5)
    scale = d ** -0.5

    def block(qi):
        s = jnp.einsum('bhcqd,bkhcd->bhcqk', qi, k, preferred_element_type=jnp.float32) * scale
        p = jax.nn.softmax(s, axis=-1)
        a = p[:, :, 0] - lam * p[:, :, 1]
        return jnp.einsum('bhqk,bkhe->bqhe', a.astype(v.dtype), v)

    o = lax.map(block, qb).transpose(1, 0, 2, 3, 4).reshape(b, t, h, DIFF_V_DIM)
    o = rms_norm(o, sub_g) * (1.0 - lambda_init)
    return o.reshape(b, t, h * DIFF_V_DIM)


def moe_ffn(h, w_router, b_router, w_in, b_in, w_out, b_out):
    b, t, dm = h.shape
    n = b * t
    xt = h.reshape(n, dm)
    logits = (xt @ w_router + b_router).astype(jnp.float32)
    top_val, top_idx = lax.top_k(logits, TOP_K)
    gates = jax.nn.softmax(top_val, axis=-1)
    n_assign = n * TOP_K
    exp_flat = top_idx.reshape(-1).astype(jnp.int32)
    tok_flat = jnp.repeat(jnp.arange(n, dtype=jnp.int32), TOP_K)
    gate_flat = gates.reshape(-1)
    order = jnp.argsort(exp_flat)
    exp_sorted = exp_flat[order]
    counts = jax.ops.segment_sum(jnp.ones_like(exp_flat), exp_flat, num_segments=N_EXPERTS)
    starts = jnp.cumsum(counts) - counts
    padded = (counts + EXPERT_BLOCK - 1) // EXPERT_BLOCK * EXPERT_BLOCK
    pad_ends = jnp.cumsum(padded)
    pad_starts = pad_ends - padded
    dest = pad_starts[exp_sorted] + (jnp.arange(n_assign, dtype=jnp.int32) - starts[exp_sorted])
    n_blocks = -(-n_assign // EXPERT_BLOCK) + N_EXPERTS
    n_rows = n_blocks * EXPERT_BLOCK
    row_tok = jnp.full((n_rows,), n, jnp.int32).at[dest].set(tok_flat[order])
    row_gate = jnp.zeros((n_rows,), jnp.float32).at[dest].set(gate_flat[order])
    block_start = jnp.arange(n_blocks, dtype=jnp.int32) * EXPERT_BLOCK
    block_exp = jnp.minimum(jnp.searchsorted(pad_ends, block_start, side='right'), N_EXPERTS - 1)
    x_pad = jnp.concatenate([xt, jnp.zeros((1, dm), xt.dtype)], axis=0)
    xs = x_pad[row_tok].reshape(n_blocks, EXPERT_BLOCK, dm)

    def expert_block(args):
        xb, e = args
        hcat = xb @ w_in[e] + b_in[e]
        x_glu = jnp.minimum(hcat[:, :D_FF], SWIGLU_LIMIT)
        x_lin = jnp.clip(hcat[:, D_FF:], -SWIGLU_LIMIT, SWIGLU_LIMIT)
        act = x_glu * jax.nn.sigmoid(SWIGLU_ALPHA * x_glu) * (x_lin + 1)
        return act @ w_out[e] + b_out[e]

    ys = lax.map(expert_block, (xs, block_exp)).reshape(n_rows, dm)
    y = jax.ops.segment_sum(ys * row_gate[:, None].astype(ys.dtype), row_tok, num_segments=n + 1)[:n]
    return y.reshape(b, t, dm)


def setup_inputs(seed: int = 0) -> dict:
    key = jax.random.key(seed)
    ks = jax.random.split(key, 21)
    f32 = jnp.float32
    nrm = lambda k, shape, s: jax.random.normal(k, shape, f32) * s
    return {
        'x': nrm(ks[0], (BATCH, SEQ, D_MODEL), 1.0),
        'c': nrm(ks[1], (BATCH, D_MODEL), 1.0),
        'ctx': nrm(ks[2], (BATCH, CTX_LEN, D_MODEL), 1.0),
        'c_ctx': nrm(ks[3], (D_MODEL,), 1.0),
        'w_ada': nrm(ks[4], (DEPTH, D_MODEL, N_MOD * D_MODEL), 0.5 * D_MODEL ** -0.5),
        'b_ada': nrm(ks[5], (DEPTH, N_MOD * D_MODEL), 0.02),
        'g_attn': 1.0 + nrm(ks[6], (DEPTH, D_MODEL), 0.05),
        'w_qkv': nrm(ks[7], (DEPTH, D_MODEL, QKV_COLS), D_MODEL ** -0.5),
        'gqa_q_norm': 1.0 + nrm(ks[8], (DEPTH, HEAD_DIM), 0.05),
        'gqa_k_norm': 1.0 + nrm(ks[9], (DEPTH, HEAD_DIM), 0.05),
        'diff_lambda': nrm(ks[10], (DEPTH, 4, HEAD_DIM), 0.1),
        'diff_subln': 1.0 + nrm(ks[11], (DEPTH, DIFF_V_DIM), 0.05),
        'w_o': nrm(ks[12], (DEPTH, MIX_WIDTH, D_MODEL), MIX_WIDTH ** -0.5),
        'g_ffn': 1.0 + nrm(ks[13], (DEPTH, D_MODEL), 0.05),
        'w_router': nrm(ks[14], (DEPTH, D_MODEL, N_EXPERTS), D_MODEL ** -0.5),
        'b_router': nrm(ks[15], (DEPTH, N_EXPERTS), 0.01),
        'w_in': nrm(ks[16], (DEPTH, N_EXPERTS, D_MODEL, 2 * D_FF), D_MODEL ** -0.5),
        'b_in': nrm(ks[17], (DEPTH, N_EXPERTS, 2 * D_FF), 0.02),
        'w_out': nrm(ks[18], (DEPTH, N_EXPERTS, D_FF, D_MODEL), D_FF ** -0.5),
        'b_out': nrm(ks[19], (DEPTH, N_EXPERTS, D_MODEL), 0.02),
        'g_final': 1.0 + nrm(ks[20], (D_MODEL,), 0.05),
    }


def reference(x, c, ctx, c_ctx, w_ada, b_ada, g_attn, w_qkv, gqa_q_norm, gqa_k_norm, diff_lambda,
              diff_subln, w_o, g_ffn, w_router, b_router, w_in, b_in, w_out, b_out, g_final):
    n_tok = x.shape[1]
    cos, sin = axial_rope_tables(n_tok)
    for l in range(DEPTH):
        lambda_init = 0.8 - 0.6 * math.exp(-0.3 * l)
        mod_x = (jax.nn.silu(c) @ w_ada[l] + b_ada[l])[:, None, :]
        mod_c = (jax.nn.silu(c_ctx) @ w_ada[l] + b_ada[l])[None, None, :]
        sh1, sc1, gt1, sh2, sc2, gt2 = jnp.split(mod_x, N_MOD, axis=-1)
        csh1, csc1, cgt1, csh2, csc2, cgt2 = jnp.split(mod_c, N_MOD, axis=-1)

        h = modulate(rms_norm(x, g_attn[l]), sh1, sc1)
        hc = modulate(rms_norm(ctx, g_attn[l]), csh1, csc1)
        qa, ka, va, qb, kb, vb = split_projection(h @ w_qkv[l])
        qa_c, ka_c, va_c, qb_c, kb_c, vb_c = split_projection(hc @ w_qkv[l])
        qa = apply_axial_rope(rms_norm(qa, gqa_q_norm[l]), cos, sin)
        ka = apply_axial_rope(rms_norm(ka, gqa_k_norm[l]), cos, sin)
        ka_c = rms_norm(ka_c, gqa_k_norm[l])
        qb = rope_diff(qb, cos, sin)
        kb = rope_diff(kb, cos, sin)
        lamf = diff_lambda[l].astype(jnp.float32)
        lam = jnp.exp(jnp.sum(lamf[0] * lamf[1])) - jnp.exp(jnp.sum(lamf[2] * lamf[3])) + lambda_init
        ka_all = jnp.concatenate([ka_c, ka], axis=1)
        va_all = jnp.concatenate([va_c, va], axis=1)
        kb_all = jnp.concatenate([kb_c, kb], axis=1)
        vb_all = jnp.concatenate([vb_c, vb], axis=1)
        lat_mix = jnp.concatenate([
            gqa_attention(qa, ka_all, va_all, Q_BLOCK),
            diff_attention(qb, kb_all, vb_all, lam, diff_subln[l], lambda_init, Q_BLOCK),
        ], axis=-1)
        x = x + gt1 * (lat_mix @ w_o[l])
        x = x + gt2 * moe_ffn(modulate(rms_norm(x, g_ffn[l]), sh2, sc2),
                              w_router[l], b_router[l], w_in[l], b_in[l], w_out[l], b_out[l])

        if l < DEPTH - 1:
            qa_c = rms_norm(qa_c, gqa_q_norm[l])
            ctx_len = ctx.shape[1]
            ctx_mix = jnp.concatenate([
                gqa_attention(qa_c, ka_c, va_c, ctx_len),
                diff_attention(qb_c, kb_c, vb_c, lam, diff_subln[l], lambda_init, ctx_len),
            ], axis=-1)
            ctx = ctx + cgt1 * (ctx_mix @ w_o[l])
            ctx = ctx + cgt2 * moe_ffn(modulate(rms_norm(ctx, g_ffn[l]), csh2, csc2),
                                       w_router[l], b_router[l], w_in[l], b_in[l], w_out[l], b_out[l])
    return rms_norm(x, g_final)
```

```python
import math
import numpy as np
import concourse.bass as bass
import concourse.mybir as mybir
from concourse.bass_utils import run_bass_kernel_spmd

F32 = mybir.dt.float32
BF16 = mybir.dt.bfloat16
I32 = mybir.dt.int32
U32 = mybir.dt.uint32
AF = mybir.ActivationFunctionType
ALU = mybir.AluOpType
AX = mybir.AxisListType

NK = 8448
NQ = 2048
NKT = NK // 128
CAP = 768
SUB = 384
NE = 32
TRASH = NE * CAP
EPS = 1e-6
TWO_PI = 2.0 * math.pi
NO_POOL_COMPUTE = False


class R:
    __slots__ = ("ap", "w", "r", "name")

    def __init__(self, ap, name=""):
        self.ap = ap
        self.w = None
        self.r = {}
        self.name = name


class Prog:
    CE = ("pe", "act", "dve", "pool")

    def __init__(self):
        self.q = {e: [] for e in ("pe", "act", "dve", "pool", "sp")}
        self.cnt = {e: 0 for e in self.CE}
        self.dcnt = {}

    def _deps(self, reads, writes, extra):
        d = {}

        def add(t):
            if t is not None and d.get(t[0], 0) < t[1]:
                d[t[0]] = t[1]

        for b in reads:
            add(b.w)
        for b in writes:
            add(b.w)
            for k, v in b.r.items():
                add((k, v))
        for t in extra:
            add(t)
        return d

    def _reg(self, tok, reads, writes):
        for b in reads:
            if b.r.get(tok[0], 0) < tok[1]:
                b.r[tok[0]] = tok[1]
        for b in writes:
            b.w = tok
            b.r = {}

    def op(self, eng, fn, reads=(), writes=(), extra=()):
        if eng == "pool" and NO_POOL_COMPUTE:
            eng = "dve"
        if eng == "gp":
            eng = "pool"
        d = self._deps(reads, writes, extra)
        if eng == "pe":
            d.pop("pe", None)
        self.cnt[eng] += 1
        tok = (eng, self.cnt[eng])
        self.q[eng].append((fn, d, tok))
        self._reg(tok, reads, writes)
        return tok

    def pe_group(self, fns, reads=(), writes=(), extra=()):
        d = self._deps(reads, writes, extra)
        d.pop("pe", None)
        self.cnt["pe"] += 1
        tok = ("pe", self.cnt["pe"])
        for i, fn in enumerate(fns):
            self.q["pe"].append((fn, d if i == 0 else {}, tok if i == len(fns) - 1 else None))
        self._reg(tok, reads, writes)
        return tok

    def dma(self, issuer, fn, sem, reads=(), writes=(), extra=()):
        d = self._deps(reads, writes, extra)
        key = "d:" + sem
        self.dcnt[key] = self.dcnt.get(key, 0) + 16
        tok = (key, self.dcnt[key])
        self.q[issuer].append((fn, d, tok))
        self._reg(tok, reads, writes)
        return tok

    def barrier(self):
        snap = dict(self.cnt)
        snap.update(self.dcnt)
        snap = {k: v for k, v in snap.items() if v > 0}
        for e in self.q:
            self.q[e].append((None, dict(snap), None))

    def wait_all(self, eng, toks):
        d = {}
        for t in toks:
            if t is not None and d.get(t[0], 0) < t[1]:
                d[t[0]] = t[1]
        self.q[eng].append((None, d, None))

    def plan(self):
        self.needed = {e: set() for e in self.CE}
        self.plans = {}
        for name, q in self.q.items():
            waited = {}
            drained = 0
            own = 0
            plan = []
            for fn, d, tok in q:
                waits = []
                do_drain = False
                for s, v in d.items():
                    if s == name and name in ("act", "dve"):
                        if v > drained:
                            do_drain = True
                        continue
                    if s == name and name == "pe":
                        continue
                    if waited.get(s, 0) < v:
                        waited[s] = v
                        waits.append((s, v))
                        if s in self.needed:
                            self.needed[s].add(v)
                if do_drain:
                    drained = own
                plan.append((waits, do_drain))
                if fn is not None and tok is not None and tok[0] == name:
                    own = tok[1]
            self.plans[name] = plan
        self.rank = {e: {v: i + 1 for i, v in enumerate(sorted(self.needed[e]))} for e in self.CE}

    def emit(self, name, eng, sems):
        for (fn, d, tok), (waits, do_drain) in zip(self.q[name], self.plans[name]):
            for s, v in waits:
                eng.wait_ge(sems[s], self.rank[s][v] if s in self.rank else v)
            if do_drain:
                eng.drain()
            if fn is None:
                continue
            ins = fn(eng)
            if tok is not None:
                if tok[0].startswith("d:"):
                    ins.then_inc(sems[tok[0]], 16)
                elif tok[1] in self.needed[tok[0]]:
                    ins.then_inc(sems[tok[0]], 1)


def ts(out, in0, s1, s2, op0, op1=None):
    if op1 is None:
        return lambda e: e.tensor_scalar(out=out, in0=in0, scalar1=s1, scalar2=None, op0=op0)
    return lambda e: e.tensor_scalar(out=out, in0=in0, scalar1=s1, scalar2=s2, op0=op0, op1=op1)


def tt(out, a, b, op):
    return lambda e: e.tensor_tensor(out=out, in0=a, in1=b, op=op)


def stt(out, in0, sc, in1, op0, op1):
    return lambda e: e.scalar_tensor_tensor(out=out, in0=in0, scalar=sc, in1=in1, op0=op0, op1=op1)


def actf(out, in_, func, bias=None, scale=None, accum=None):
    kw = {}
    if bias is not None:
        kw["bias"] = bias
    if scale is not None:
        kw["scale"] = scale
    if accum is not None:
        kw["accum_out"] = accum
    return lambda e: e.activation(out=out, in_=in_, func=func, **kw)


def cp(out, in_):
    return lambda e: e.tensor_copy(out=out, in_=in_)


def mm(out, lhsT, rhs, start, stop, **kw):
    return lambda e: e.matmul(out, lhsT, rhs, start=start, stop=stop, **kw)


def trp(out, in_, ident):
    return lambda e: e.transpose(out, in_, ident)


def dmaf(out, in_):
    return lambda e: e.dma_start(out=out, in_=in_)


def mset(ap, v):
    return lambda e: e.memset(ap, v)


class Arena:
    def __init__(self, t, nwords):
        self.t = t
        self.n = nwords
        self.off = 0

    def _take(self, nw):
        assert self.off + nw <= self.n, ("arena overflow", self.off, nw, self.n)
        v = self.t[:, self.off:self.off + nw]
        self.off += nw
        return v

    def alloc(self, shape, dt=F32, name=""):
        n = int(np.prod(shape[1:]))
        if dt == BF16:
            nw = (n + 1) // 2
            v = self._take(nw).bitcast(BF16)[:, 0:n]
        elif dt == F32:
            v = self._take(n)
        else:
            v = self._take(n).bitcast(dt)
        if len(shape) == 3:
            v = v.rearrange("p (a b) -> p a b", a=shape[1])
        elif len(shape) == 4:
            v = v.rearrange("p (a b c) -> p a b c", a=shape[1], b=shape[2])
        if shape[0] != 128:
            v = v[0:shape[0]]
        return R(v, name)


def build_nc(stage=99):
    nc = bass.Bass("TRN2", target_bir_lowering=False)
    D = {}

    def din(name, shape, dt=F32):
        D[name] = nc.dram_tensor(name, list(shape), dt, kind="ExternalInput").ap()

    din("xkv", [NK, 1024]); din("xq", [NQ, 1024]); din("cvec", [128, 16]); din("pos0", [128, 1])
    din("pmeta", [128, 4]); din("w_ada", [1024, 6144]); din("b_ada", [6144]); din("g_attn", [1024])
    din("wkv", [1024, 1920]); din("wq", [1024, 2048]); din("gq", [128, 2]); din("gk", [128, 2])
    din("dlam", [256]); din("subln", [128]); din("w_o", [1024, 1024]); din("g_ffn", [1024])
    if stage >= 3:
        din("w_r", [1024, 32]); din("b_r", [32]); din("w_in", [NE, 1024, 2048]); din("b_in", [NE, 128, 16])
        din("w_out", [NE, 1024, 1024]); din("b_out", [NE, 1024]); din("g_fin", [1024])
    out_d = nc.dram_tensor("out", [NQ, 1024], F32, kind="ExternalOutput").ap()
    kT_scr = R(nc.dram_tensor("kT_scr", [5, 128, NK], BF16, kind="Internal").ap())
    v_scr = R(nc.dram_tensor("v_scr", [NK, 640], BF16, kind="Internal").ap())
    xs_scr = R(nc.dram_tensor("xs_scr", [TRASH + 128, 1024], BF16, kind="Internal").ap())
    ys_scr = R(nc.dram_tensor("ys_scr", [TRASH + 128, 1024], F32, kind="Internal").ap())
    dbg = {}
    if stage < 99:
        dbg["kT"] = nc.dram_tensor("dbg_kT", [5, 128, NK], BF16, kind="ExternalOutput").ap()
        dbg["v"] = nc.dram_tensor("dbg_v", [NK, 640], BF16, kind="ExternalOutput").ap()
        dbg["qT"] = nc.dram_tensor("dbg_qT", [128, 8, NQ], BF16, kind="ExternalOutput").ap()
        dbg["lat"] = nc.dram_tensor("dbg_lat", [128, 16, 1024], BF16, kind="ExternalOutput").ap()
        dbg["x1"] = nc.dram_tensor("dbg_x1", [128, 16, 1024], F32, kind="ExternalOutput").ap()
        dbg["rowi"] = nc.dram_tensor("dbg_rowi", [128, 16, 4], I32, kind="ExternalOutput").ap()
        dbg["gk"] = nc.dram_tensor("dbg_gk", [128, 16, 4], F32, kind="ExternalOutput").ap()
        dbg["mod"] = nc.dram_tensor("dbg_mod", [128, 8, 1024], F32, kind="ExternalOutput").ap()

    NW = 51200
    P = Prog()
    nc.declared_inputs = list(D.keys())
    with nc.sbuf_tensor("arena", [128, NW], F32) as arena_t, nc.psum_tensor("ps", [128, 8, 512], F32) as ps_t:
        A = Arena(arena_t, NW)
        PS = [R(ps_t[:, i, :], f"ps{i}") for i in range(8)]

        def psb(i):
            return PS[i].ap.bitcast(BF16)

        ident_f = A.alloc([128, 128], F32)
        ident_b = A.alloc([128, 128], BF16)
        bones = A.alloc([128, 128], F32)
        ones_b = A.alloc([128, 128], BF16)
        ltri_b = A.alloc([128, 128], BF16)
        iota_e = A.alloc([128, 32], F32)
        pm = A.alloc([128, 4], F32)
        smalls = A.alloc([128, 64], F32)
        gq_t = A.alloc([128, 2], F32); gk_t = A.alloc([128, 2], F32)
        one_c = A.alloc([128, 2], F32)
        tmp_i = A.alloc([128, 512], I32)
        tmp_f = A.alloc([128, 512], F32)
        A0 = A.alloc([128, 512], F32)
        offs = A.alloc([128, 16], F32)
        offq = A.alloc([128, 4], F32)
        modbc = [None] * 8
        for i in range(4, 8):
            modbc[i] = A.alloc([128, 1024], F32, f"mod{i}")
        late_mark = A.off
        for i in range(0, 4):
            modbc[i] = A.alloc([128, 1024], F32, f"mod{i}")
        QT = [A.alloc([128, NQ], BF16, f"QT{g}") for g in range(8)]
        persist_mark = A.off

        invf = smalls.ap[:, 0:1]; rowf = smalls.ap[:, 1:2]; colf = smalls.ap[:, 2:3]; sgn = smalls.ap[:, 3:4]
        neglam = smalls.ap[:, 4:5]; pof = smalls.ap[:, 5:6]

        P.op("gp", lambda e: e.iota(tmp_i.ap[:, 0:128], pattern=[[1, 128]], base=0, channel_multiplier=-1), writes=[tmp_i])
        P.op("dve", ts(ident_f.ap, tmp_i.ap[:, 0:128], 0.0, None, ALU.is_equal), reads=[tmp_i], writes=[ident_f])
        P.op("dve", cp(ident_b.ap, ident_f.ap), reads=[ident_f], writes=[ident_b])
        P.op("dve", ts(ltri_b.ap, tmp_i.ap[:, 0:128], 0.0, None, ALU.is_gt), reads=[tmp_i], writes=[ltri_b])
        P.op("pool", mset(ones_b.ap, 1.0), writes=[ones_b])
        P.op("pool", mset(bones.ap, 0.0), writes=[bones])
        P.op("pool", mset(bones.ap[0:64, 0:64], 1.0), writes=[bones])
        P.op("pool", mset(bones.ap[64:128, 64:128], 1.0), writes=[bones])
        P.op("pool", mset(one_c.ap, 1.0), writes=[one_c])
        P.op("gp", lambda e: e.iota(tmp_i.ap[:, 128:160], pattern=[[1, 32]], base=0, channel_multiplier=0), writes=[tmp_i])
        P.op("dve", cp(iota_e.ap, tmp_i.ap[:, 128:160]), reads=[tmp_i], writes=[iota_e])
        P.dma("sp", dmaf(pm.ap, D["pmeta"]), "c0", writes=[pm])
        P.dma("sp", dmaf(gq_t.ap, D["gq"]), "c1", writes=[gq_t])
        P.dma("sp", dmaf(gk_t.ap, D["gk"]), "c2", writes=[gk_t])
        P.dma("sp", dmaf(smalls.ap[:, 8:9], D["pos0"]), "c3", writes=[smalls])
        P.op("act", actf(invf, pm.ap[:, 0:1], AF.Exp, scale=-math.log(10000.0) / 16.0), reads=[pm], writes=[smalls])
        P.op("dve", tt(colf, invf, pm.ap[:, 1:2], ALU.mult), reads=[smalls, pm], writes=[smalls])
        P.op("dve", tt(rowf, invf, colf, ALU.subtract), reads=[smalls], writes=[smalls])
        P.op("dve", cp(sgn, pm.ap[:, 2:3]), reads=[pm], writes=[smalls])
        P.op("dve", tt(pof, smalls.ap[:, 8:9], rowf, ALU.mult), reads=[smalls], writes=[smalls])
        P.op("gp", lambda e: e.iota(tmp_i.ap, pattern=[[1, 8], [0, 64]], base=0, channel_multiplier=0), writes=[tmp_i])
        P.op("dve", ts(A0.ap, tmp_i.ap, rowf, None, ALU.mult), reads=[tmp_i, smalls], writes=[A0])
        P.op("gp", lambda e: e.iota(tmp_i.ap, pattern=[[0, 8], [1, 64]], base=0, channel_multiplier=0), reads=[A0], writes=[tmp_i])
        P.op("dve", cp(tmp_f.ap, tmp_i.ap), reads=[tmp_i], writes=[tmp_f])
        P.op("dve", stt(A0.ap, tmp_f.ap, colf, A0.ap, ALU.mult, ALU.add), reads=[tmp_f, smalls, A0], writes=[A0])
        P.op("gp", lambda e: e.iota(tmp_i.ap[:, 0:16], pattern=[[8, 16]], base=0, channel_multiplier=0), reads=[tmp_f], writes=[tmp_i])
        P.op("dve", ts(offs.ap, tmp_i.ap[:, 0:16], rowf, None, ALU.mult), reads=[tmp_i, smalls], writes=[offs])
        P.op("dve", ts(offq.ap, offs.ap[:, 0:4], pof, None, ALU.add), reads=[offs, smalls], writes=[offq])

        mark0 = A.off
        cv = A.alloc([128, 16], F32)
        sv = A.alloc([128, 16], F32)
        svb = A.alloc([128, 16, 128], F32)
        wst = [A.alloc([128, 8, 512], F32, f"wst{i}") for i in range(2)]
        bst = [A.alloc([128, 512], F32, f"bst{i}") for i in range(2)]
        gbc = A.alloc([128, 1024], F32)
        P.dma("sp", dmaf(cv.ap, D["cvec"]), "c4", writes=[cv])
        P.op("act", actf(sv.ap, cv.ap, AF.Silu), reads=[cv], writes=[sv])
        P.op("dve", cp(svb.ap, sv.ap.unsqueeze(2).to_broadcast([128, 16, 128])), reads=[sv], writes=[svb])
        w_ada_v = D["w_ada"].rearrange("(k p) n -> p k n", p=128)
        jobs = [(0, [(0, 0, None), (1, 2, None)]), (1, [(0, 1, "g_attn"), (1, 3, "g_attn")]),
                (2, [(0, 4, None)]), (3, [(0, 5, None)]), (4, [(0, 6, "g_ffn")]), (5, [(0, 7, None)])]
        it = 0
        last_g = None
        for chunk, dests in jobs:
            for half in range(2):
                s = it % 2
                it += 1
                c0 = chunk * 1024 + half * 512
                P.dma("sp", dmaf(wst[s].ap, w_ada_v[:, :, c0:c0 + 512]), f"wst{s}", writes=[wst[s]])
                P.dma("sp", dmaf(bst[s].ap, D["b_ada"][c0:c0 + 512].partition_broadcast(128)), f"bst{s}", writes=[bst[s]])
                for (j, di, gname) in dests:
                    if gname is not None and gname != last_g:
                        P.dma("sp", dmaf(gbc.ap, D[gname].partition_broadcast(128)), "gbc", writes=[gbc])
                        last_g = gname
                    pb = PS[(it + j) % 2]
                    P.pe_group([mm(pb.ap, svb.ap[:, k * 2 + j, :], wst[s].ap[:, k, :], k == 0, k == 7) for k in range(8)],
                               reads=[svb, wst[s]], writes=[pb])
                    dst = modbc[di].ap[:, half * 512:(half + 1) * 512]
                    P.op("dve", tt(dst, pb.ap, bst[s].ap, ALU.add), reads=[pb, bst[s]], writes=[modbc[di]])
                    if gname is not None:
                        P.op("dve", stt(dst, dst, 1.0, gbc.ap[:, half * 512:(half + 1) * 512], ALU.add, ALU.mult),
                             reads=[modbc[di], gbc], writes=[modbc[di]])
        if stage == 0:
            toks = []
            for i in range(8):
                toks.append(P.dma("sp", dmaf(dbg["mod"][:, i, :], modbc[i].ap), "dbg", reads=[modbc[i]]))
            P.wait_all("sp", toks)
        P.barrier()
        A.off = mark0

        L = dict(locals())
        if stage >= 1:
            phase1(nc, P, A, PS, psb, D, L)
        if stage == 1:
            P.barrier()
            t1 = P.dma("sp", dmaf(dbg["kT"], kT_scr.ap), "dbg", reads=[kT_scr])
            t2 = P.dma("sp", dmaf(dbg["v"], v_scr.ap), "dbg", reads=[v_scr])
            toks = [t1, t2]
            for g in range(8):
                toks.append(P.dma("sp", dmaf(dbg["qT"][:, g, :], QT[g].ap), "dbg", reads=[QT[g]]))
            P.wait_all("sp", toks)
        P.barrier()
        A.off = persist_mark

        if stage >= 2:
            phase2(nc, P, A, PS, psb, D, L)
        if stage == 2:
            P.barrier()
            P.wait_all("sp", [P.dma("sp", dmaf(dbg["lat"], L["lat"].ap), "dbg", reads=[L["lat"]])])
        P.barrier()

        if stage >= 3:
            phase3(nc, P, A, PS, psb, D, L, dbg, stage, out_d)
        P.barrier()

        P.plan()
        sem_names = list(P.CE) + sorted(P.dcnt.keys())
        with nc.cleanup_on_exit():
            sems = {n: nc.alloc_semaphore("s_" + n.replace(":", "_")) for n in sem_names}
            for h in sems.values():
                nc.gpsimd.sem_clear(h)
            nc.all_engine_barrier()
            with nc.Block() as block:
                @block.tensor
                def _(e):
                    P.emit("pe", e, sems)

                @block.scalar
                def _(e):
                    P.emit("act", e, sems)

                @block.vector
                def _(e):
                    P.emit("dve", e, sems)

                @block.gpsimd
                def _(e):
                    P.emit("pool", e, sems)

                @block.sync
                def _(e):
                    P.emit("sp", e, sems)
    return nc


def phase1(nc, P, A, PS, psb, D, L):
    ident_b = L["ident_b"]; bones = L["bones"]; modbc = L["modbc"]; QT = L["QT"]
    A0 = L["A0"]; offs = L["offs"]; offq = L["offq"]; smalls = L["smalls"]
    gq_t = L["gq_t"]; gk_t = L["gk_t"]; kT_scr = L["kT_scr"]; v_scr = L["v_scr"]
    sgn = L["sgn"]
    xin = [A.alloc([128, 1024], F32, f"xin{i}") for i in range(3)]
    junk = A.alloc([128, 1024], BF16)
    st = [A.alloc([128, 4], F32, f"st{i}") for i in range(3)]
    t1 = [A.alloc([128, 1024], F32, f"t1{i}") for i in range(2)]
    hb = [A.alloc([128, 1024], BF16, f"hb{i}") for i in range(2)]
    hT = [A.alloc([128, 8, 512], BF16, f"hT{i}") for i in range(2)]
    wbuf = A.alloc([128, 8, 2048], BF16)
    Ct = [A.alloc([128, 512], F32, f"Ct{i}") for i in range(2)]
    St = [A.alloc([128, 512], F32, f"St{i}") for i in range(2)]
    ang = A.alloc([128, 512], F32); angi = A.alloc([128, 512], I32); red = A.alloc([128, 512], F32)
    sq = A.alloc([128, 512], F32); kg = A.alloc([128, 512], F32); krg = A.alloc([128, 512], F32)
    ta = [A.alloc([128, 512], F32, f"ta{i}") for i in range(2)]
    tb = [A.alloc([128, 512], F32, f"tb{i}") for i in range(2)]
    rbc = A.alloc([128, 512], F32)
    kout = [[A.alloc([128, 512], BF16, f"ko{g}{i}") for i in range(2)] for g in range(5)]
    vst = [A.alloc([128, 4, 640], BF16, f"vst{i}") for i in range(2)]
    TP = PS[0]; KP = [PS[1], PS[2]]; KRP = [PS[3], PS[4]]; VP = [PS[5], PS[6]]; SSB = PS[7]

    def make_tables(slot, off_ap, off_r):
        for (dst, shift, scl) in ((St[slot], 0.0, sgn), (Ct[slot], math.pi / 2, None)):
            P.op("dve", ts(ang.ap, A0.ap, off_ap, shift, ALU.add, ALU.add), reads=[A0, off_r], writes=[ang])
            P.op("dve", ts(angi.ap, ang.ap, 1.0 / TWO_PI, None, ALU.mult), reads=[ang], writes=[angi])
            P.op("dve", stt(red.ap, angi.ap, -TWO_PI, ang.ap, ALU.mult, ALU.add), reads=[angi, ang], writes=[red])
            P.op("dve", ts(red.ap, red.ap, 3.14159, -3.14159, ALU.min, ALU.max), reads=[red], writes=[red])
            if scl is None:
                P.op("act", actf(dst.ap, red.ap, AF.Sin), reads=[red], writes=[dst])
            else:
                P.op("act", actf(dst.ap, red.ap, AF.Sin, scale=scl), reads=[red, smalls], writes=[dst])

    def ident_tables(slot):
        P.op("pool", mset(Ct[slot].ap, 1.0), writes=[Ct[slot]])
        P.op("pool", mset(St[slot].ap, 0.0), writes=[St[slot]])

    state = {"xi": 0, "ti": 0, "gi": 0, "ko": 0}

    def token_tile_a(src_ap, sh_r, gm_r):
        i = state["xi"]; state["xi"] += 1
        xs = xin[i % 3]; s_ = st[i % 3]; t_ = t1[i % 2]; h_ = hb[i % 2]
        P.dma("sp", dmaf(xs.ap, src_ap), f"xin{i % 3}", writes=[xs])
        P.op("act", actf(junk.ap, xs.ap, AF.Square, accum=s_.ap[:, 0:1]), reads=[xs], writes=[junk, s_])
        P.op("act", actf(s_.ap[:, 1:2], s_.ap[:, 0:1], AF.Ln, bias=EPS, scale=1.0 / 1024.0), reads=[s_], writes=[s_])
        P.op("act", actf(s_.ap[:, 2:3], s_.ap[:, 1:2], AF.Exp, scale=-0.5), reads=[s_], writes=[s_])
        P.op("dve", stt(t_.ap, xs.ap, s_.ap[:, 2:3], gm_r.ap, ALU.mult, ALU.mult), reads=[xs, s_, gm_r], writes=[t_])
        P.op("pool", tt(h_.ap, t_.ap, sh_r.ap, ALU.add), reads=[t_, sh_r], writes=[h_])
        return h_

    def token_tile_b(h_, hT_r, col0):
        P.pe_group([trp(psb(0)[:, k * 128:(k + 1) * 128], h_.ap[:, k * 128:(k + 1) * 128], ident_b.ap) for k in range(8)],
                   reads=[h_, ident_b], writes=[TP])
        P.op("act", cp_act(hT_r.ap[:, :, col0:col0 + 128], psb(0).rearrange("p (k n) -> p k n", k=8)),
             reads=[TP], writes=[hT_r])

    def token_tile(src_ap, sh_r, gm_r, hT_r, col0):
        token_tile_b(token_tile_a(src_ap, sh_r, gm_r), hT_r, col0)

    def cp_act(out, in_):
        return lambda e: e.activation(out=out, in_=in_, func=AF.Copy)

    def qk_features(hT_r, ntok, ngroups, wcol0, rotcol0, g_t, norm_groups, tslot, dest_fn, between=(), vtiles=()):
        held = {}
        for g in range(ngroups):
            j = state["gi"] % 2; state["gi"] += 1
            kp, krp = KP[j], KRP[j]
            if g < len(between):
                held[g] = between[g][0]()
            P.pe_group([mm(kp.ap[:, 0:ntok], wbuf.ap[:, k, wcol0 + g * 128: wcol0 + (g + 1) * 128], hT_r.ap[:, k, 0:ntok], k == 0, k == 7)
                        for k in range(8)], reads=[wbuf, hT_r], writes=[kp])
            P.pe_group([mm(krp.ap[:, 0:ntok], wbuf.ap[:, k, rotcol0 + g * 128: rotcol0 + (g + 1) * 128], hT_r.ap[:, k, 0:ntok], k == 0, k == 7)
                        for k in range(8)], reads=[wbuf, hT_r], writes=[krp])
            if (g - 1) in held:
                between[g - 1][1](held.pop(g - 1))
            if g == ngroups - 1 and g in held:
                between[g][1](held.pop(g))
            if g < len(vtiles):
                vtiles[g]()
            dst_r, dst_ap, after = dest_fn(g)
            ta_, tb_ = ta[j], tb[j]
            C_, S_ = Ct[tslot], St[tslot]
            if g in norm_groups:
                P.op("act", actf(sq.ap[:, 0:ntok], kp.ap[:, 0:ntok], AF.Square), reads=[kp], writes=[sq])
                P.pe_group([mm(SSB.ap[:, 0:ntok], bones.ap, sq.ap[:, 0:ntok], True, True)], reads=[bones, sq], writes=[SSB])
                P.op("act", actf(rbc.ap[:, 0:ntok], SSB.ap[:, 0:ntok], AF.Ln, bias=EPS, scale=1.0 / 64.0), reads=[SSB], writes=[rbc])
                P.op("act", actf(rbc.ap[:, 0:ntok], rbc.ap[:, 0:ntok], AF.Exp, scale=-0.5), reads=[rbc], writes=[rbc])
                P.op("act", actf(kg.ap[:, 0:ntok], kp.ap[:, 0:ntok], AF.Copy, scale=g_t.ap[:, 0:1]), reads=[kp, g_t], writes=[kg])
                P.op("act", actf(krg.ap[:, 0:ntok], krp.ap[:, 0:ntok], AF.Copy, scale=g_t.ap[:, 1:2]), reads=[krp, g_t], writes=[krg])
                P.op("dve", tt(ta_.ap[:, 0:ntok], kg.ap[:, 0:ntok], C_.ap[:, 0:ntok], ALU.mult), reads=[kg, C_], writes=[ta_])
                P.op("pool", tt(tb_.ap[:, 0:ntok], krg.ap[:, 0:ntok], S_.ap[:, 0:ntok], ALU.mult), reads=[krg, S_], writes=[tb_])
                P.op("dve", tt(ta_.ap[:, 0:ntok], ta_.ap[:, 0:ntok], tb_.ap[:, 0:ntok], ALU.add), reads=[ta_, tb_], writes=[ta_])
                P.op("dve", tt(dst_ap, ta_.ap[:, 0:ntok], rbc.ap[:, 0:ntok], ALU.mult), reads=[ta_, rbc], writes=[dst_r])
            else:
                P.op("dve", tt(ta_.ap[:, 0:ntok], kp.ap[:, 0:ntok], C_.ap[:, 0:ntok], ALU.mult), reads=[kp, C_], writes=[ta_])
                P.op("dve", tt(tb_.ap[:, 0:ntok], krp.ap[:, 0:ntok], S_.ap[:, 0:ntok], ALU.mult), reads=[krp, S_], writes=[tb_])
                P.op("pool", tt(dst_ap, ta_.ap[:, 0:ntok], tb_.ap[:, 0:ntok], ALU.add), reads=[ta_, tb_], writes=[dst_r])
            if after is not None:
                after()

    wkv_v = D["wkv"].rearrange("(k p) n -> p k n", p=128)
    for hlf in range(2):
        P.dma("pool", dmaf(wbuf.ap[:, hlf * 4:(hlf + 1) * 4, 0:1920], wkv_v[:, hlf * 4:(hlf + 1) * 4, :]), "wbuf", writes=[wbuf])
    store_toks = []
    ngrp = 17
    work = []
    for grp in range(ngrp):
        ntok = 256 if grp == 0 else 512
        tok0 = 0 if grp == 0 else 256 + (grp - 1) * 512
        work.append(dict(kind="kv", grp=grp, ntok=ntok, tok0=tok0, src=D["xkv"], off=(None if grp == 0 else (offs, grp - 1)),
                         mods=(modbc[2], modbc[3]) if grp == 0 else (modbc[0], modbc[1])))
    for qg in range(4):
        work.append(dict(kind="q", grp=ngrp + qg, qg=qg, ntok=512, tok0=qg * 512, src=D["xq"], off=(L["offq"], qg), mods=(modbc[0], modbc[1])))

    def prep_tables(w):
        tslot = w["grp"] % 2
        if w["off"] is None:
            ident_tables(tslot)
        else:
            r_, c_ = w["off"]
            make_tables(tslot, r_.ap[:, c_:c_ + 1], r_)

    def tile_fn(w, t):
        def fa():
            return token_tile_a(w["src"][w["tok0"] + t * 128: w["tok0"] + (t + 1) * 128, :], w["mods"][0], w["mods"][1])

        def fb(h_):
            token_tile_b(h_, hT[w["grp"] % 2], t * 128)
        return (fa, fb)

    prep_tables(work[0])
    for t in range(work[0]["ntok"] // 128):
        fa, fb = tile_fn(work[0], t)
        fb(fa())
    wq_v = D["wq"].rearrange("(k p) n -> p k n", p=128)
    for wi, w in enumerate(work):
        grp, ntok, tok0 = w["grp"], w["ntok"], w["tok0"]
        hT_r = hT[grp % 2]
        tslot = grp % 2
        between = []
        if wi + 1 < len(work):
            nw = work[wi + 1]
            prep_tables(nw)
            between = [tile_fn(nw, t) for t in range(nw["ntok"] // 128)]
        if w["kind"] == "kv":
            def dest_fn(g, grp=grp, ntok=ntok, tok0=tok0):
                ko = kout[g][grp % 2]

                def after():
                    store_toks.append(P.dma("pool", dmaf(kT_scr.ap[g, :, tok0:tok0 + ntok], ko.ap[:, 0:ntok]), f"ko{g}{grp % 2}",
                                            reads=[ko], writes=[]))
                return ko, ko.ap[:, 0:ntok], after

            vs_ = vst[grp % 2]

            def vtile(t, hT_r=hT_r, vs_=vs_):
                def f():
                    P.pe_group([mm(VP[0].ap, hT_r.ap[:, k, t * 128:(t + 1) * 128], wbuf.ap[:, k, 1280:1792], k == 0, k == 7) for k in range(8)] +
                               [mm(VP[1].ap[:, 0:128], hT_r.ap[:, k, t * 128:(t + 1) * 128], wbuf.ap[:, k, 1792:1920], k == 0, k == 7) for k in range(8)],
                               reads=[hT_r, wbuf], writes=[VP[0], VP[1]])
                    P.op("act", cp_act(vs_.ap[:, t, 0:512], VP[0].ap), reads=[VP[0]], writes=[vs_])
                    P.op("dve", cp(vs_.ap[:, t, 512:640], VP[1].ap[:, 0:128]), reads=[VP[1]], writes=[vs_])
                return f

            qk_features(hT_r, ntok, 5, 0, 640, gk_t, (0,), tslot, dest_fn, between=between[:4],
                        vtiles=[vtile(t) for t in range(ntok // 128)])
            nt = ntok // 128
            store_toks.append(P.dma("pool", dmaf(v_scr.ap[tok0:tok0 + ntok, :].rearrange("(t p) n -> p t n", p=128), vs_.ap[:, 0:nt, :]),
                                    f"vst{grp % 2}", reads=[vs_], writes=[]))
            if grp == ngrp - 1:
                for hlf in range(2):
                    P.dma("pool", dmaf(wbuf.ap[:, hlf * 4:(hlf + 1) * 4, :], wq_v[:, hlf * 4:(hlf + 1) * 4, :]), "wbuf", writes=[wbuf])
        else:
            qg = w["qg"]

            def dest_fn(g, qg=qg):
                return QT[g], QT[g].ap[:, qg * 512:(qg + 1) * 512], None

            qk_features(hT_r, 512, 8, 0, 1024, L["gq_t"], (0, 1, 2, 3), tslot, dest_fn, between=between)
    L["store_toks"] = store_toks


def phase2(nc, P, A, PS, psb, D, L):
    QT = L["QT"]; kT_scr = L["kT_scr"]; v_scr = L["v_scr"]; ps_t = L["ps_t"]; smalls = L["smalls"]
    neglam = L["neglam"]
    lat = A.alloc([128, 16, 1024], BF16, "lat")
    L["lat"] = lat
    L["lat_end"] = A.off
    KT = [A.alloc([128, NK], BF16, f"KT{i}") for i in range(2)]
    VV = [A.alloc([128, NKT, 132], BF16, f"VV{i}") for i in range(2)]
    PT = [A.alloc([128, 1024], BF16, f"PT{i}") for i in range(3)]
    rec = [A.alloc([128, 4], F32, f"rec{i}") for i in range(2)]
    o1 = A.alloc([128, 4, 128], F32); o2 = A.alloc([128, 4, 128], F32)
    dd = A.alloc([128, 4, 128], F32); sqd = A.alloc([128, 4, 128], F32)
    ssd = A.alloc([128, 8], F32)
    subbc = A.alloc([128, 128], F32)
    dl = A.alloc([128, 256], F32); dl2 = A.alloc([128, 256], F32); ls = A.alloc([128, 4], F32)
    zt2 = A.alloc([128, 1024], F32, "zt2")
    S = [[PS[0], PS[1]], [PS[2], PS[3]]]
    Oset = [R(None, "O0"), R(None, "O1")]

    def Ov(m):
        return ps_t[:, 4 + 2 * m:6 + 2 * m, :].rearrange("p b (j c) -> p (b j) c", j=2)

    P.dma("sp", dmaf(dl.ap, D["dlam"].partition_broadcast(128)), "dl", writes=[dl])
    P.dma("sp", dmaf(subbc.ap, D["subln"].partition_broadcast(128)), "sub", writes=[subbc])
    P.op("dve", ts(subbc.ap, subbc.ap, 0.8, None, ALU.mult), reads=[subbc], writes=[subbc])
    P.op("dve", tt(dl2.ap[:, 0:64], dl.ap[:, 0:64], dl.ap[:, 64:128], ALU.mult), reads=[dl], writes=[dl2])
    P.op("dve", tt(dl2.ap[:, 64:128], dl.ap[:, 128:192], dl.ap[:, 192:256], ALU.mult), reads=[dl], writes=[dl2])
    P.op("dve", lambda e: e.reduce_sum(out=ls.ap[:, 0:2], in_=dl2.ap[:, 0:128].rearrange("p (a b) -> p a b", a=2), axis=AX.X),
         reads=[dl2], writes=[ls])
    P.op("act", actf(ls.ap[:, 2:4], ls.ap[:, 0:2], AF.Exp), reads=[ls], writes=[ls])
    P.op("dve", tt(neglam, ls.ap[:, 3:4], ls.ap[:, 2:3], ALU.subtract), reads=[ls], writes=[smalls])
    P.op("dve", ts(neglam, neglam, -0.2, None, ALU.add), reads=[smalls], writes=[smalls])
    for s_ in range(2):
        P.op("pool", mset(VV[s_].ap[:, :, 0:1], 1.0), writes=[VV[s_]])

    groups = [("A", 0), ("A", 1), ("B", 0), ("B", 1), ("B", 2), ("B", 3)]

    def load_group(gi):
        kind, j = groups[gi]
        slot = gi % 2
        if kind == "A":
            for half in range(2):
                P.dma("sp", dmaf(KT[slot].ap[half * 64:(half + 1) * 64, :], kT_scr.ap[0, j * 64:(j + 1) * 64, :]), f"KT{slot}",
                      reads=[kT_scr], writes=[KT[slot]])
            c0, dv = j * 64, 64
        else:
            P.dma("sp", dmaf(KT[slot].ap, kT_scr.ap[1 + j]), f"KT{slot}", reads=[kT_scr], writes=[KT[slot]])
            c0, dv = 128 + j * 128, 128
        vv = v_scr.ap[:, c0:c0 + dv].rearrange("(t p) n -> p t n", p=128)
        for part in range(6):
            P.dma("sp", dmaf(VV[slot].ap[:, part * 11:(part + 1) * 11, 1:1 + dv], vv[:, part * 11:(part + 1) * 11, :]), f"VV{slot}",
                  reads=[v_scr], writes=[VV[slot]])

    iters = []
    for gi, (kind, j) in enumerate(groups):
        for qt in range(4):
            if kind == "A":
                for pr in range(2):
                    iters.append(dict(gi=gi, qt=qt, kind="A", fg=2 * j + pr, dv=64, hq0=4 * j + 2 * pr))
            else:
                iters.append(dict(gi=gi, qt=qt, kind="B", fg=4 + j, dv=128, h=j))
    steps = [(ii, kt) for ii in range(len(iters)) for kt in range(NKT)]

    def emit_qk(i):
        ii, kt = steps[i]
        itd = iters[ii]
        slot = itd["gi"] % 2
        p = i % 2
        qs_ = slice(itd["qt"] * 512, (itd["qt"] + 1) * 512)
        P.pe_group([mm(S[p][m].ap, KT[slot].ap[m * 64:(m + 1) * 64, kt * 128:(kt + 1) * 128],
                       QT[itd["fg"]].ap[m * 64:(m + 1) * 64, qs_], True, True) for m in range(2)],
                   reads=[KT[slot], QT[itd["fg"]]], writes=[S[p][0], S[p][1]])

    def emit_exp(i):
        p = i % 2
        sv = ps_t[:, 2 * p:2 * p + 2, :].rearrange("p b c -> p (b c)")
        P.op("act", actf(PT[i % 3].ap, sv, AF.Exp, scale=0.125), reads=[S[p][0], S[p][1]], writes=[PT[i % 3]])

    def emit_pv(i):
        ii, kt = steps[i]
        itd = iters[ii]
        slot = itd["gi"] % 2
        dv = itd["dv"]
        fns = []
        for m in range(2):
            ov = Ov(m)
            pt = PT[i % 3]
            for j in range(4):
                kw = {"skip_group_check": True} if j % 2 == 1 else {}
                fns.append(mm(ov[:, j, 0:dv + 1], pt.ap[:, m * 512 + j * 128:m * 512 + (j + 1) * 128], VV[slot].ap[:, kt, 0:dv + 1],
                              (kt == 0 and j % 2 == 0), kt == NKT - 1, **kw))
        P.pe_group(fns, reads=[PT[i % 3], VV[slot]], writes=[Oset[0], Oset[1]])
        if kt == NKT - 1:
            evac(ii)

    def evac(ii):
        itd = iters[ii]
        qt = itd["qt"]
        dv = itd["dv"]
        for m in range(2):
            ov = Ov(m)
            rc = rec[m]
            P.op("dve", lambda e, rc=rc, ov=ov: e.reciprocal(out=rc.ap.unsqueeze(2), in_=ov[:, :, 0:1]), reads=[Oset[m]], writes=[rc])
            rbc_ = rc.ap.unsqueeze(2).to_broadcast([128, 4, dv])
            if itd["kind"] == "A":
                hq = itd["hq0"] + m
                P.op("dve", tt(lat.ap[:, qt * 4:(qt + 1) * 4, hq * 64:(hq + 1) * 64], ov[:, :, 1:65], rbc_, ALU.mult),
                     reads=[Oset[m], rc], writes=[lat])
            else:
                om = o1 if m == 0 else o2
                P.op("dve", tt(om.ap, ov[:, :, 1:129], rbc_, ALU.mult), reads=[Oset[m], rc], writes=[om])
        if itd["kind"] == "A":
            return
        h = itd["h"]
        P.op("dve", stt(dd.ap, o2.ap, neglam, o1.ap, ALU.mult, ALU.add), reads=[o2, o1, smalls], writes=[dd])
        P.op("pool", tt(sqd.ap, dd.ap, dd.ap, ALU.mult), reads=[dd], writes=[sqd])
        P.op("dve", lambda e: e.reduce_sum(out=ssd.ap[:, 0:4], in_=sqd.ap, axis=AX.X), reads=[sqd], writes=[ssd])
        P.op("act", actf(ssd.ap[:, 4:8], ssd.ap[:, 0:4], AF.Ln, bias=EPS, scale=1.0 / 128.0), reads=[ssd], writes=[ssd])
        P.op("act", actf(ssd.ap[:, 4:8], ssd.ap[:, 4:8], AF.Exp, scale=-0.5), reads=[ssd], writes=[ssd])
        P.op("dve", tt(dd.ap, dd.ap, ssd.ap[:, 4:8].unsqueeze(2).to_broadcast([128, 4, 128]), ALU.mult), reads=[dd, ssd], writes=[dd])
        P.op("dve", tt(lat.ap[:, qt * 4:(qt + 1) * 4, 512 + h * 128:512 + (h + 1) * 128], dd.ap,
                       subbc.ap.unsqueeze(1).to_broadcast([128, 4, 128]), ALU.mult), reads=[dd, subbc], writes=[lat])

    load_group(0)
    load_group(1)
    xs_scr = L["xs_scr"]; ys_scr = L["ys_scr"]
    P.op("pool", mset(zt2.ap, 0.0), writes=[zt2])
    P.dma("sp", dmaf(ys_scr.ap[TRASH:TRASH + 128, :], zt2.ap), "zt", reads=[zt2], writes=[ys_scr])
    ztb = zt2.ap.bitcast(BF16)
    nfull = (TRASH + 128) // 256
    for r in range(nfull):
        P.dma("sp", dmaf(xs_scr.ap[r * 256:(r + 1) * 256, :].rearrange("(p a) d -> p (a d)", a=2), ztb), "zf", reads=[zt2], writes=[xs_scr])
    rem = (TRASH + 128) - nfull * 256
    if rem:
        P.dma("sp", dmaf(xs_scr.ap[nfull * 256:nfull * 256 + rem, :], ztb[0:rem, 0:1024]), "zf", reads=[zt2], writes=[xs_scr])
    n = len(steps)
    emit_qk(0)
    for i in range(n):
        ii, kt = steps[i]
        if kt == 0 and ii > 0 and iters[ii]["gi"] != iters[ii - 1]["gi"]:
            gnext = iters[ii]["gi"] + 1
            if gnext < len(groups):
                load_group(gnext)
        if i + 1 < n:
            emit_qk(i + 1)
        emit_exp(i)
        emit_pv(i)


def phase3(nc, P, A, PS, psb, D, L, dbg, stage, out_d):
    ident_b = L["ident_b"]; ident_f = L["ident_f"]; modbc = L["modbc"]; lat = L["lat"]; ones_b = L["ones_b"]
    ltri_b = L["ltri_b"]; iota_e = L["iota_e"]; xs_scr = L["xs_scr"]; ys_scr = L["ys_scr"]
    arena_t = L["arena_t"]
    x1_scr = R(nc.dram_tensor("x1_scr", [NQ, 1024], F32, kind="Internal").ap())
    A2 = Arena(arena_t, L["persist_mark"])
    A2.off = L["late_mark"]
    A.off = L["lat_end"]
    rowi = A2.alloc([128, 16, 4], I32, "rowi")
    gkk = A2.alloc([128, 16, 4], F32, "gkk")
    base = A2.alloc([128, 32], F32, "base")
    wr = A2.alloc([128, 8, 32], F32, "wr")
    brbc = A2.alloc([128, 32], F32, "brbc")
    zt = A2.alloc([128, 1024], F32, "zt")
    sm = [A2.alloc([128, 64], F32, f"sm{i}") for i in range(2)]
    smi = [A2.alloc([128, 8], U32, f"smi{i}") for i in range(2)]
    a2_mark = A2.off
    mark3 = A.off
    wo = A.alloc([128, 8, 1024], BF16, "wo")
    lmT = [A.alloc([128, 8, 128], BF16, f"lmT{i}") for i in range(2)]
    xt = [A.alloc([128, 1024], F32, f"xt{i}") for i in range(2)]
    tmp = [A.alloc([128, 1024], F32, f"tmp{i}") for i in range(2)]
    x1t = [A.alloc([128, 1024], F32, f"x1t{i}") for i in range(2)]
    h2f = [A.alloc([128, 1024], F32, f"h2f{i}") for i in range(2)]
    h2b = [A.alloc([128, 1024], BF16, f"h2b{i}") for i in range(2)]
    junk = A.alloc([128, 1024], BF16, "junk3")
    h2T = A2.alloc([128, 8, 128], F32, "h2T")
    lg = [A2.alloc([128, 32], F32, f"lg{i}") for i in range(2)]
    oh = A2.alloc([128, 32], F32, "oh")
    pos = [A2.alloc([128, 32], F32, f"pos{i}") for i in range(2)]
    maskb = [A2.alloc([128, 32], BF16, f"maskb{i}") for i in range(2)]
    TP = PS[0]; PO = [PS[1], PS[2]]; HT = [PS[3], PS[4]]; LG = PS[5]; PP = PS[6]; PC = PS[7]

    P.dma("pool", dmaf(wo.ap, D["w_o"].rearrange("(k p) n -> p k n", p=128)), "wo", writes=[wo])
    P.dma("sp", dmaf(wr.ap, D["w_r"].rearrange("(k p) n -> p k n", p=128)), "wr", writes=[wr])
    P.dma("sp", dmaf(brbc.ap, D["b_r"].partition_broadcast(128)), "brbc", writes=[brbc])
    P.op("pool", mset(base.ap, 0.0), writes=[base])
    P.op("pool", mset(zt.ap, 0.0), writes=[zt])
    x1_toks = []
    for i in range(16):
        s_ = i % 2
        P.dma("sp", dmaf(xt[s_].ap, D["xq"][i * 128:(i + 1) * 128, :]), f"xt{s_}", writes=[xt[s_]])
        P.pe_group([trp(psb(0)[:, k * 128:(k + 1) * 128], lat.ap[:, i, k * 128:(k + 1) * 128], ident_b.ap) for k in range(8)],
                   reads=[lat, ident_b], writes=[TP])
        P.op("act", lambda e, o=lmT[s_].ap: e.activation(out=o, in_=psb(0).rearrange("p (k n) -> p k n", k=8), func=AF.Copy),
             reads=[TP], writes=[lmT[s_]])
        for h in range(2):
            P.pe_group([mm(PO[h].ap, lmT[s_].ap[:, k, :], wo.ap[:, k, h * 512:(h + 1) * 512], k == 0, k == 7) for k in range(8)],
                       reads=[lmT[s_], wo], writes=[PO[h]])
            P.op("dve", tt(tmp[s_].ap[:, h * 512:(h + 1) * 512], PO[h].ap, modbc[4].ap[:, h * 512:(h + 1) * 512], ALU.mult),
                 reads=[PO[h], modbc[4]], writes=[tmp[s_]])
        P.op("dve", tt(x1t[s_].ap, tmp[s_].ap, xt[s_].ap, ALU.add), reads=[tmp[s_], xt[s_]], writes=[x1t[s_]])
        x1_toks.append(P.dma("pool", dmaf(x1_scr.ap[i * 128:(i + 1) * 128, :], x1t[s_].ap), f"x1s{s_}", reads=[x1t[s_]]))
        st_ = sm[s_]
        P.op("act", actf(junk.ap, x1t[s_].ap, AF.Square, accum=st_.ap[:, 0:1]), reads=[x1t[s_]], writes=[junk, st_])
        P.op("act", actf(st_.ap[:, 1:2], st_.ap[:, 0:1], AF.Ln, bias=EPS, scale=1.0 / 1024.0), reads=[st_], writes=[st_])
        P.op("act", actf(st_.ap[:, 2:3], st_.ap[:, 1:2], AF.Exp, scale=-0.5), reads=[st_], writes=[st_])
        P.op("dve", stt(tmp[s_].ap, x1t[s_].ap, st_.ap[:, 2:3], modbc[6].ap, ALU.mult, ALU.mult), reads=[x1t[s_], st_, modbc[6]], writes=[tmp[s_]])
        P.op("dve", tt(h2f[s_].ap, tmp[s_].ap, modbc[5].ap, ALU.add), reads=[tmp[s_], modbc[5]], writes=[h2f[s_]])
        P.op("act", lambda e, o=h2b[s_].ap, a=h2f[s_].ap: e.activation(out=o, in_=a, func=AF.Copy), reads=[h2f[s_]], writes=[h2b[s_]])
        for hh in range(2):
            P.pe_group([trp(HT[hh].ap[:, k * 128:(k + 1) * 128], h2f[s_].ap[:, (hh * 4 + k) * 128:(hh * 4 + k + 1) * 128], ident_f.ap)
                        for k in range(4)], reads=[h2f[s_], ident_f], writes=[HT[hh]])
        P.op("act", lambda e: e.activation(out=h2T.ap[:, 0:4, :], in_=HT[0].ap.rearrange("p (k n) -> p k n", k=4), func=AF.Copy),
             reads=[HT[0]], writes=[h2T])
        P.op("dve", cp(h2T.ap[:, 4:8, :], HT[1].ap.rearrange("p (k n) -> p k n", k=4)), reads=[HT[1]], writes=[h2T])
        P.pe_group([mm(LG.ap[:, 0:32], h2T.ap[:, k, :], wr.ap[:, k, :], k == 0, k == 7) for k in range(8)], reads=[h2T, wr], writes=[LG])
        lg_ = lg[s_]; mi_ = smi[s_]
        P.op("dve", tt(lg_.ap, LG.ap[:, 0:32], brbc.ap, ALU.add), reads=[LG, brbc], writes=[lg_])
        mx8 = st_.ap[:, 8:16]
        P.op("dve", lambda e, o=mx8, a=lg_.ap: e.max(out=o, in_=a), reads=[lg_], writes=[st_])
        P.op("dve", lambda e, o=mi_.ap, m=mx8, a=lg_.ap: e.max_index(out=o, in_max=m, in_values=a), reads=[lg_, st_], writes=[mi_])
        P.op("dve", ts(maskb[s_].ap, lg_.ap, st_.ap[:, 11:12], None, ALU.is_ge), reads=[lg_, st_], writes=[maskb[s_]])
        P.op("dve", ts(st_.ap[:, 16:17], st_.ap[:, 8:9], -1.0, None, ALU.mult), reads=[st_], writes=[st_])
        P.op("act", actf(st_.ap[:, 20:24], st_.ap[:, 8:12], AF.Exp, bias=st_.ap[:, 16:17]), reads=[st_], writes=[st_])
        P.op("dve", lambda e, o=st_.ap[:, 24:25], a=st_.ap[:, 20:24]: e.reduce_sum(out=o, in_=a, axis=AX.X), reads=[st_], writes=[st_])
        P.op("dve", lambda e, o=st_.ap[:, 25:26], a=st_.ap[:, 24:25]: e.reciprocal(out=o, in_=a), reads=[st_], writes=[st_])
        P.op("dve", ts(gkk.ap[:, i, :], st_.ap[:, 20:24], st_.ap[:, 25:26], None, ALU.mult), reads=[st_], writes=[gkk])
        P.pe_group([mm(PP.ap[:, 0:32], ltri_b.ap, maskb[s_].ap, True, True)], reads=[ltri_b, maskb[s_]], writes=[PP])
        P.pe_group([mm(PC.ap[:, 0:32], ones_b.ap, maskb[s_].ap, True, True)], reads=[ones_b, maskb[s_]], writes=[PC])
        pos_ = pos[s_]
        P.op("dve", tt(pos_.ap, PP.ap[:, 0:32], base.ap, ALU.add), reads=[PP, base], writes=[pos_])
        P.op("dve", tt(base.ap, PC.ap[:, 0:32], base.ap, ALU.add), reads=[PC, base], writes=[base])
        P.op("dve", cp(st_.ap[:, 28:32], mi_.ap[:, 0:4]), reads=[mi_], writes=[st_])
        for k in range(4):
            P.op("dve", ts(oh.ap, iota_e.ap, st_.ap[:, 28 + k:29 + k], None, ALU.is_equal), reads=[iota_e, st_], writes=[oh])
            P.op("dve", tt(oh.ap, oh.ap, pos_.ap, ALU.mult), reads=[oh, pos_], writes=[oh])
            P.op("dve", lambda e, k=k, st_=st_: e.reduce_sum(out=st_.ap[:, 32 + k:33 + k], in_=oh.ap, axis=AX.X), reads=[oh], writes=[st_])
        P.op("dve", stt(st_.ap[:, 36:40], st_.ap[:, 28:32], float(CAP), st_.ap[:, 32:36], ALU.mult, ALU.add), reads=[st_], writes=[st_])
        P.op("dve", ts(st_.ap[:, 40:44], st_.ap[:, 32:36], float(CAP), None, ALU.is_ge), reads=[st_], writes=[st_])
        P.op("dve", ts(st_.ap[:, 44:48], st_.ap[:, 36:40], -1.0, float(TRASH), ALU.mult, ALU.add), reads=[st_], writes=[st_])
        P.op("dve", tt(st_.ap[:, 44:48], st_.ap[:, 44:48], st_.ap[:, 40:44], ALU.mult), reads=[st_], writes=[st_])
        P.op("dve", tt(st_.ap[:, 36:40], st_.ap[:, 36:40], st_.ap[:, 44:48], ALU.add), reads=[st_], writes=[st_])
        P.op("dve", cp(rowi.ap[:, i, :], st_.ap[:, 36:40]), reads=[st_], writes=[rowi])
        for k in range(4 if stage != 3 else 0):
            P.dma("pool", lambda e, i=i, k=k, s_=s_: e.indirect_dma_start(
                out=xs_scr.ap, out_offset=bass.IndirectOffsetOnAxis(ap=rowi.ap[:, i, k:k + 1], axis=0),
                in_=h2b[s_].ap, in_offset=None), f"sc{s_}", reads=[rowi, h2b[s_]], writes=[xs_scr])
    if stage == 3:
        P.barrier()
        P.wait_all("sp", [P.dma("sp", dmaf(dbg["x1"], x1_scr.ap.rearrange("(t p) n -> p t n", p=128)), "dbg"),
                          P.dma("sp", dmaf(dbg["rowi"], rowi.ap), "dbg"), P.dma("sp", dmaf(dbg["gk"], gkk.ap), "dbg")])
        return
    P.barrier()
    mark3 = a2_mark
    A.off = mark3

    win = [A.alloc([128, 8, 2048], BF16, f"win{i}") for i in range(2)]
    wout = [A.alloc([128, 8, 1024], BF16, f"wout{i}") for i in range(2)]
    xs = [A.alloc([128, 3, 1024], BF16, f"xs{i}") for i in range(2)]
    xsT = [A.alloc([128, 8, SUB], BF16, f"xsT{i}") for i in range(2)]
    actT = A.alloc([128, 8, SUB], BF16, "actT")
    gg = [A.alloc([128, SUB], F32, f"gg{i}") for i in range(2)]
    sg = [A.alloc([128, SUB], F32, f"sg{i}") for i in range(2)]
    ll = [A.alloc([128, SUB], F32, f"ll{i}") for i in range(2)]
    ysst = A.alloc([128, 3, 1024], F32, "ysst")
    binb = [A.alloc([128, 16], F32, f"binb{i}") for i in range(2)]
    boutbc = [A.alloc([128, 1024], F32, f"boutbc{i}") for i in range(2)]
    PG = [PS[1], PS[2]]; PL = [PS[3], PS[4]]; PY = [PS[5], PS[6]]
    w_in_v = D["w_in"].rearrange("e (k p) n -> e p k n", p=128)
    w_out_v = D["w_out"].rearrange("e (k p) n -> e p k n", p=128)
    nslot = SUB // 128

    def load_w(e):
        s_ = e % 2
        for hlf in range(2):
            P.dma("pool", dmaf(win[s_].ap[:, hlf * 4:(hlf + 1) * 4, :], w_in_v[e][:, hlf * 4:(hlf + 1) * 4, :]), f"win{s_}", writes=[win[s_]])
        P.dma("pool", dmaf(wout[s_].ap, w_out_v[e]), f"wout{s_}", writes=[wout[s_]])
        P.dma("sp", dmaf(binb[s_].ap, D["b_in"][e]), f"binb{s_}", writes=[binb[s_]])
        P.dma("sp", dmaf(boutbc[s_].ap, D["b_out"][e].partition_broadcast(128)), f"boutbc{s_}", writes=[boutbc[s_]])

    load_w(0)
    passes = [(e_, sb_) for e_ in range(NE) for sb_ in range(CAP // SUB)]
    TPs = [(PS[0], 0), (PS[7], 7)]
    tpc = [0]

    def load_xs(pi):
        e_, sb_ = passes[pi]
        r0_ = e_ * CAP + sb_ * SUB
        P.dma("sp", dmaf(xs[pi % 2].ap[:, 0:nslot, :], xs_scr.ap[r0_:r0_ + SUB, :].rearrange("(s p) d -> p s d", p=128)), f"xs{pi % 2}",
              reads=[xs_scr], writes=[xs[pi % 2]])

    load_xs(0)
    for pi, (e, sub) in enumerate(passes):
        ws_ = e % 2
        s_ = pi % 2
        r0 = e * CAP + sub * SUB
        if sub == 0 and e + 1 < NE:
            load_w(e + 1)
        if pi + 1 < len(passes):
            load_xs(pi + 1)
        for sl in range(nslot):
            tpr, tpi = TPs[tpc[0] % 2]
            tpc[0] += 1
            P.pe_group([trp(psb(tpi)[:, k * 128:(k + 1) * 128], xs[s_].ap[:, sl, k * 128:(k + 1) * 128], ident_b.ap) for k in range(8)],
                       reads=[xs[s_], ident_b], writes=[tpr])
            P.op("act", lambda e_, o=xsT[s_].ap[:, :, sl * 128:(sl + 1) * 128], tpi=tpi: e_.activation(
                out=o, in_=psb(tpi).rearrange("p (k n) -> p k n", k=8), func=AF.Copy), reads=[tpr], writes=[xsT[s_]])
        for j in range(8):
            b_ = j % 2
            P.pe_group([mm(PG[b_].ap[:, 0:SUB], win[ws_].ap[:, k, j * 128:(j + 1) * 128], xsT[s_].ap[:, k, :], k == 0, k == 7) for k in range(8)],
                       reads=[win[ws_], xsT[s_]], writes=[PG[b_]])
            P.pe_group([mm(PL[b_].ap[:, 0:SUB], win[ws_].ap[:, k, 1024 + j * 128:1024 + (j + 1) * 128], xsT[s_].ap[:, k, :], k == 0, k == 7)
                        for k in range(8)], reads=[win[ws_], xsT[s_]], writes=[PL[b_]])
            P.op("dve", ts(gg[b_].ap, PG[b_].ap[:, 0:SUB], binb[ws_].ap[:, j:j + 1], 7.0, ALU.add, ALU.min), reads=[PG[b_], binb[ws_]], writes=[gg[b_]])
            P.op("act", actf(sg[b_].ap, gg[b_].ap, AF.Sigmoid, scale=1.702), reads=[gg[b_]], writes=[sg[b_]])
            P.op("dve", ts(ll[b_].ap, PL[b_].ap[:, 0:SUB], binb[ws_].ap[:, 8 + j:9 + j], 7.0, ALU.add, ALU.min), reads=[PL[b_], binb[ws_]], writes=[ll[b_]])
            P.op("dve", ts(ll[b_].ap, ll[b_].ap, -7.0, 1.0, ALU.max, ALU.add), reads=[ll[b_]], writes=[ll[b_]])
            P.op("pool", tt(gg[b_].ap, gg[b_].ap, sg[b_].ap, ALU.mult), reads=[gg[b_], sg[b_]], writes=[gg[b_]])
            P.op("pool", tt(actT.ap[:, j, :], gg[b_].ap, ll[b_].ap, ALU.mult), reads=[gg[b_], ll[b_]], writes=[actT])
        for sl in range(nslot):
            for h in range(2):
                py = PY[(sl * 2 + h) % 2]
                P.pe_group([mm(py.ap, actT.ap[:, j, sl * 128:(sl + 1) * 128], wout[ws_].ap[:, j, h * 512:(h + 1) * 512], j == 0, j == 7) for j in range(8)],
                           reads=[actT, wout[ws_]], writes=[py])
                P.op("dve", tt(ysst.ap[:, sl, h * 512:(h + 1) * 512], py.ap, boutbc[ws_].ap[:, h * 512:(h + 1) * 512], ALU.add),
                     reads=[py, boutbc[ws_]], writes=[ysst])
        P.dma("sp", dmaf(ys_scr.ap[r0:r0 + SUB, :].rearrange("(s p) d -> p s d", p=128), ysst.ap[:, 0:nslot, :]), "ysst",
              reads=[ysst], writes=[ys_scr])
    P.barrier()
    A.off = mark3

    yg = [[A.alloc([128, 1024], F32, f"yg{i}{k}") for k in range(4)] for i in range(2)]
    x1l = [A.alloc([128, 1024], F32, f"x1l{i}") for i in range(2)]
    acc = [A.alloc([128, 1024], F32, f"acc{i}") for i in range(2)]
    ot = [A.alloc([128, 1024], F32, f"ot{i}") for i in range(2)]
    gfin = A.alloc([128, 1024], F32, "gfin")
    junk2 = A.alloc([128, 1024], BF16, "junk4")
    P.dma("sp", dmaf(gfin.ap, D["g_fin"].partition_broadcast(128)), "gfin", writes=[gfin])
    out_toks = []
    for i in range(16):
        s_ = i % 2
        st_ = sm[s_]
        P.dma("sp", dmaf(x1l[s_].ap, x1_scr.ap[i * 128:(i + 1) * 128, :]), f"x1l{s_}", reads=[x1_scr], writes=[x1l[s_]])
        for k in range(4):
            P.dma("pool", lambda e, i=i, k=k, s_=s_: e.indirect_dma_start(
                out=yg[s_][k].ap, out_offset=None, in_=ys_scr.ap,
                in_offset=bass.IndirectOffsetOnAxis(ap=rowi.ap[:, i, k:k + 1], axis=0)), f"yg{s_}{k}",
                reads=[rowi, ys_scr], writes=[yg[s_][k]])
        P.op("dve", ts(acc[s_].ap, yg[s_][0].ap, gkk.ap[:, i, 0:1], None, ALU.mult), reads=[yg[s_][0], gkk], writes=[acc[s_]])
        for k in range(1, 4):
            P.op("dve", stt(acc[s_].ap, yg[s_][k].ap, gkk.ap[:, i, k:k + 1], acc[s_].ap, ALU.mult, ALU.add),
                 reads=[yg[s_][k], gkk, acc[s_]], writes=[acc[s_]])
        P.op("dve", tt(acc[s_].ap, acc[s_].ap, modbc[7].ap, ALU.mult), reads=[acc[s_], modbc[7]], writes=[acc[s_]])
        P.op("dve", tt(acc[s_].ap, acc[s_].ap, x1l[s_].ap, ALU.add), reads=[acc[s_], x1l[s_]], writes=[acc[s_]])
        P.op("act", actf(junk2.ap, acc[s_].ap, AF.Square, accum=st_.ap[:, 0:1]), reads=[acc[s_]], writes=[junk2, st_])
        P.op("act", actf(st_.ap[:, 1:2], st_.ap[:, 0:1], AF.Ln, bias=EPS, scale=1.0 / 1024.0), reads=[st_], writes=[st_])
        P.op("act", actf(st_.ap[:, 2:3], st_.ap[:, 1:2], AF.Exp, scale=-0.5), reads=[st_], writes=[st_])
        P.op("dve", stt(ot[s_].ap, acc[s_].ap, st_.ap[:, 2:3], gfin.ap, ALU.mult, ALU.mult), reads=[acc[s_], st_, gfin], writes=[ot[s_]])
        out_toks.append(P.dma("sp", dmaf(out_d[i * 128:(i + 1) * 128, :], ot[s_].ap), f"ot{s_}", reads=[ot[s_]]))
    P.wait_all("sp", out_toks)


def _partner_perm():
    j = np.arange(64)
    a, h, f = j // 32, (j % 32) // 16, j % 16
    return a * 32 + (1 - h) * 16 + f


_NC_CACHE = {}


def prepare_inputs(x, c, ctx, c_ctx, w_ada, b_ada, g_attn, w_qkv, gqa_q_norm, gqa_k_norm, diff_lambda,
                   diff_subln, w_o, g_ffn, w_router, b_router, w_in, b_in, w_out, b_out, g_final):
    f = lambda a: np.ascontiguousarray(np.asarray(a, dtype=np.float32))
    x, c, ctx, c_ctx = f(x), f(c), f(ctx), f(c_ctx)
    wqkv = f(w_qkv)[0]
    part = _partner_perm()

    def rot_cols(w, nheads):
        idx = (np.arange(nheads)[:, None] * 64 + part[None, :]).reshape(-1)
        return w[:, idx]

    qa, ka, va = wqkv[:, 0:512], wqkv[:, 512:640], wqkv[:, 640:768]
    qb, kb, vb = wqkv[:, 768:1280], wqkv[:, 1280:1792], wqkv[:, 1792:2304]
    wkv = np.ascontiguousarray(np.concatenate([ka, kb, rot_cols(ka, 2), rot_cols(kb, 8), va, vb], axis=1))
    wq = np.ascontiguousarray(np.concatenate([qa, qb, rot_cols(qa, 8), rot_cols(qb, 8)], axis=1))
    p = np.arange(128)
    j = p % 64
    gqn, gkn = f(gqa_q_norm)[0], f(gqa_k_norm)[0]
    gq = np.ascontiguousarray(np.stack([gqn[j], gqn[part[j]]], axis=1))
    gk = np.ascontiguousarray(np.stack([gkn[j], gkn[part[j]]], axis=1))
    pmeta = np.zeros((128, 4), np.float32)
    pmeta[:, 0] = j % 16
    pmeta[:, 1] = j // 32
    pmeta[:, 2] = np.where((j % 32) // 16 == 0, -1.0, 1.0)
    b_in_l = np.ascontiguousarray(f(b_in)[0].reshape(NE, 16, 128).transpose(0, 2, 1))
    shared = dict(pmeta=pmeta, w_ada=f(w_ada)[0], b_ada=f(b_ada)[0], g_attn=f(g_attn)[0], wkv=wkv, wq=wq, gq=gq, gk=gk,
                  dlam=f(diff_lambda)[0].reshape(256), subln=f(diff_subln)[0], w_o=f(w_o)[0], g_ffn=f(g_ffn)[0],
                  w_r=f(w_router)[0], b_r=f(b_router)[0], w_in=f(w_in)[0], b_in=b_in_l, w_out=f(w_out)[0],
                  b_out=f(b_out)[0], g_fin=f(g_final))
    in_maps = []
    for core in range(8):
        b, qi = core // 4, core % 4
        m = dict(shared)
        m["xkv"] = np.ascontiguousarray(np.concatenate([ctx[b], x[b]], axis=0))
        m["xq"] = np.ascontiguousarray(x[b, qi * NQ:(qi + 1) * NQ])
        cv = np.stack([c[b], c_ctx], axis=1)
        m["cvec"] = np.ascontiguousarray(cv.reshape(8, 128, 2).transpose(1, 0, 2).reshape(128, 16))
        m["pos0"] = np.full((128, 1), float(qi * NQ // 64), np.float32)
        in_maps.append(m)
    return in_maps


def kernel(**inputs):
    in_maps = prepare_inputs(**inputs)
    if "nc" not in _NC_CACHE:
        _NC_CACHE["nc"] = build_nc()
    res = run_bass_kernel_spmd(_NC_CACHE["nc"], in_maps, core_ids=list(range(8)))
    out = np.zeros((2, 8192, 1024), np.float32)
    for core in range(8):
        b, qi = core // 4, core % 4
        out[b, qi * NQ:(qi + 1) * NQ] = res.results[core]["out"]
    return out
```

```python
import math
import numpy as np
import concourse.bass as bass
import concourse.mybir as mybir
from concourse.bass_utils import run_bass_kernel_spmd

F32 = mybir.dt.float32
BF16 = mybir.dt.bfloat16
I32 = mybir.dt.int32
U32 = mybir.dt.uint32
AF = mybir.ActivationFunctionType
ALU = mybir.AluOpType
AX = mybir.AxisListType

NK = 8448
NQ = 2048
NKT = NK // 128
CAP = 768
SUB = 384
NE = 32
TRASH = NE * CAP
EPS = 1e-6
TWO_PI = 2.0 * math.pi
NO_POOL_COMPUTE = False


class R:
    __slots__ = ("ap", "w", "r", "name")

    def __init__(self, ap, name=""):
        self.ap = ap
        self.w = None
        self.r = {}
        self.name = name


class Prog:
    CE = ("pe", "act", "dve", "pool")

    def __init__(self):
        self.q = {e: [] for e in ("pe", "act", "dve", "pool", "sp")}
        self.cnt = {e: 0 for e in self.CE}
        self.dcnt = {}

    def _deps(self, reads, writes, extra):
        d = {}

        def add(t):
            if t is not None and d.get(t[0], 0) < t[1]:
                d[t[0]] = t[1]

        for b in reads:
            add(b.w)
        for b in writes:
            add(b.w)
            for k, v in b.r.items():
                add((k, v))
        for t in extra:
            add(t)
        return d

    def _reg(self, tok, reads, writes):
        for b in reads:
            if b.r.get(tok[0], 0) < tok[1]:
                b.r[tok[0]] = tok[1]
        for b in writes:
            b.w = tok
            b.r = {}

    def op(self, eng, fn, reads=(), writes=(), extra=()):
        if eng == "pool" and NO_POOL_COMPUTE:
            eng = "dve"
        if eng == "gp":
            eng = "pool"
        d = self._deps(reads, writes, extra)
        if eng == "pe":
            d.pop("pe", None)
        self.cnt[eng] += 1
        tok = (eng, self.cnt[eng])
        self.q[eng].append((fn, d, tok))
        self._reg(tok, reads, writes)
        return tok

    def pe_group(self, fns, reads=(), writes=(), extra=()):
        d = self._deps(reads, writes, extra)
        d.pop("pe", None)
        self.cnt["pe"] += 1
        tok = ("pe", self.cnt["pe"])
        for i, fn in enumerate(fns):
            self.q["pe"].append((fn, d if i == 0 else {}, tok if i == len(fns) - 1 else None))
        self._reg(tok, reads, writes)
        return tok

    def dma(self, issuer, fn, sem, reads=(), writes=(), extra=()):
        d = self._deps(reads, writes, extra)
        key = "d:" + sem
        self.dcnt[key] = self.dcnt.get(key, 0) + 16
        tok = (key, self.dcnt[key])
        self.q[issuer].append((fn, d, tok))
        self._reg(tok, reads, writes)
        return tok

    def barrier(self):
        snap = dict(self.cnt)
        snap.update(self.dcnt)
        snap = {k: v for k, v in snap.items() if v > 0}
        for e in self.q:
            self.q[e].append((None, dict(snap), None))

    def wait_all(self, eng, toks):
        d = {}
        for t in toks:
            if t is not None and d.get(t[0], 0) < t[1]:
                d[t[0]] = t[1]
        self.q[eng].append((None, d, None))

    def plan(self):
        self.needed = {e: set() for e in self.CE}
        self.plans = {}
        for name, q in self.q.items():
            waited = {}
            drained = 0
            own = 0
            plan = []
            for fn, d, tok in q:
                waits = []
                do_drain = False
                for s, v in d.items():
                    if s == name and name in ("act", "dve"):
                        if v > drained:
                            do_drain = True
                        continue
                    if s == name and name == "pe":
                        continue
                    if waited.get(s, 0) < v:
                        waited[s] = v
                        waits.append((s, v))
                        if s in self.needed:
                            self.needed[s].add(v)
                if do_drain:
                    drained = own
                plan.append((waits, do_drain))
                if fn is not None and tok is not None and tok[0] == name:
                    own = tok[1]
            self.plans[name] = plan
        self.rank = {e: {v: i + 1 for i, v in enumerate(sorted(self.needed[e]))} for e in self.CE}

    def emit(self, name, eng, sems):
        for (fn, d, tok), (waits, do_drain) in zip(self.q[name], self.plans[name]):
            for s, v in waits:
                eng.wait_ge(sems[s], self.rank[s][v] if s in self.rank else v)
            if do_drain:
                eng.drain()
            if fn is None:
                continue
            ins = fn(eng)
            if tok is not None:
                if tok[0].startswith("d:"):
                    ins.then_inc(sems[tok[0]], 16)
                elif tok[1] in self.needed[tok[0]]:
                    ins.then_inc(sems[tok[0]], 1)


def ts(out, in0, s1, s2, op0, op1=None):
    if op1 is None:
        return lambda e: e.tensor_scalar(out=out, in0=in0, scalar1=s1, scalar2=None, op0=op0)
    return lambda e: e.tensor_scalar(out=out, in0=in0, scalar1=s1, scalar2=s2, op0=op0, op1=op1)


def tt(out, a, b, op):
    return lambda e: e.tensor_tensor(out=out, in0=a, in1=b, op=op)


def stt(out, in0, sc, in1, op0, op1):
    return lambda e: e.scalar_tensor_tensor(out=out, in0=in0, scalar=sc, in1=in1, op0=op0, op1=op1)


def actf(out, in_, func, bias=None, scale=None, accum=None):
    kw = {}
    if bias is not None:
        kw["bias"] = bias
    if scale is not None:
        kw["scale"] = scale
    if accum is not None:
        kw["accum_out"] = accum
    return lambda e: e.activation(out=out, in_=in_, func=func, **kw)


def cp(out, in_):
    return lambda e: e.tensor_copy(out=out, in_=in_)


def mm(out, lhsT, rhs, start, stop, **kw):
    return lambda e: e.matmul(out, lhsT, rhs, start=start, stop=stop, **kw)


def trp(out, in_, ident):
    return lambda e: e.transpose(out, in_, ident)


def dmaf(out, in_):
    return lambda e: e.dma_start(out=out, in_=in_)


def mset(ap, v):
    return lambda e: e.memset(ap, v)


class Arena:
    def __init__(self, t, nwords):
        self.t = t
        self.n = nwords
        self.off = 0

    def _take(self, nw):
        assert self.off + nw <= self.n, ("arena overflow", self.off, nw, self.n)
        v = self.t[:, self.off:self.off + nw]
        self.off += nw
        return v

    def alloc(self, shape, dt=F32, name=""):
        n = int(np.prod(shape[1:]))
        if dt == BF16:
            nw = (n + 1) // 2
            v = self._take(nw).bitcast(BF16)[:, 0:n]
        elif dt == F32:
            v = self._take(n)
        else:
            v = self._take(n).bitcast(dt)
        if len(shape) == 3:
            v = v.rearrange("p (a b) -> p a b", a=shape[1])
        elif len(shape) == 4:
            v = v.rearrange("p (a b c) -> p a b c", a=shape[1], b=shape[2])
        if shape[0] != 128:
            v = v[0:shape[0]]
        return R(v, name)


def build_nc(stage=99):
    nc = bass.Bass("TRN2", target_bir_lowering=False)
    D = {}

    def din(name, shape, dt=F32):
        D[name] = nc.dram_tensor(name, list(shape), dt, kind="ExternalInput").ap()

    din("xkv", [NK, 1024]); din("xq", [NQ, 1024]); din("cvec", [128, 16]); din("pos0", [128, 1])
    din("pmeta", [128, 4]); din("w_ada", [1024, 6144]); din("b_ada", [6144]); din("g_attn", [1024])
    din("wkv", [1024, 1920]); din("wq", [1024, 2048]); din("gq", [128, 2]); din("gk", [128, 2])
    din("dlam", [256]); din("subln", [128]); din("w_o", [1024, 1024]); din("g_ffn", [1024])
    if stage >= 3:
        din("w_r", [1024, 32]); din("b_r", [32]); din("w_in", [NE, 1024, 2048]); din("b_in", [NE, 128, 16])
        din("w_out", [NE, 1024, 1024]); din("b_out", [NE, 1024]); din("g_fin", [1024])
    out_d = nc.dram_tensor("out", [NQ, 1024], F32, kind="ExternalOutput").ap()
    kT_scr = R(nc.dram_tensor("kT_scr", [5, 128, NK], BF16, kind="Internal").ap())
    v_scr = R(nc.dram_tensor("v_scr", [NK, 640], BF16, kind="Internal").ap())
    xs_scr = R(nc.dram_tensor("xs_scr", [TRASH + 128, 1024], BF16, kind="Internal").ap())
    ys_scr = R(nc.dram_tensor("ys_scr", [TRASH + 128, 1024], F32, kind="Internal").ap())
    dbg = {}
    if stage < 99:
        dbg["kT"] = nc.dram_tensor("dbg_kT", [5, 128, NK], BF16, kind="ExternalOutput").ap()
        dbg["v"] = nc.dram_tensor("dbg_v", [NK, 640], BF16, kind="ExternalOutput").ap()
        dbg["qT"] = nc.dram_tensor("dbg_qT", [128, 8, NQ], BF16, kind="ExternalOutput").ap()
        dbg["lat"] = nc.dram_tensor("dbg_lat", [128, 16, 1024], BF16, kind="ExternalOutput").ap()
        dbg["x1"] = nc.dram_tensor("dbg_x1", [128, 16, 1024], F32, kind="ExternalOutput").ap()
        dbg["rowi"] = nc.dram_tensor("dbg_rowi", [128, 16, 4], I32, kind="ExternalOutput").ap()
        dbg["gk"] = nc.dram_tensor("dbg_gk", [128, 16, 4], F32, kind="ExternalOutput").ap()
        dbg["mod"] = nc.dram_tensor("dbg_mod", [128, 8, 1024], F32, kind="ExternalOutput").ap()

    NW = 51200
    P = Prog()
    nc.declared_inputs = list(D.keys())
    with nc.sbuf_tensor("arena", [128, NW], F32) as arena_t, nc.psum_tensor("ps", [128, 8, 512], F32) as ps_t:
        A = Arena(arena_t, NW)
        PS = [R(ps_t[:, i, :], f"ps{i}") for i in range(8)]

        def psb(i):
            return PS[i].ap.bitcast(BF16)

        ident_f = A.alloc([128, 128], F32)
        ident_b = A.alloc([128, 128], BF16)
        bones = A.alloc([128, 128], F32)
        ones_b = A.alloc([128, 128], BF16)
        ltri_b = A.alloc([128, 128], BF16)
        iota_e = A.alloc([128, 32], F32)
        pm = A.alloc([128, 4], F32)
        smalls = A.alloc([128, 64], F32)
        gq_t = A.alloc([128, 2], F32); gk_t = A.alloc([128, 2], F32)
        one_c = A.alloc([128, 2], F32)
        tmp_i = A.alloc([128, 512], I32)
        tmp_f = A.alloc([128, 512], F32)
        A0 = A.alloc([128, 512], F32)
        offs = A.alloc([128, 16], F32)
        offq = A.alloc([128, 4], F32)
        modbc = [None] * 8
        for i in range(4, 8):
            modbc[i] = A.alloc([128, 1024], F32, f"mod{i}")
        late_mark = A.off
        for i in range(0, 4):
            modbc[i] = A.alloc([128, 1024], F32, f"mod{i}")
        QT = [A.alloc([128, NQ], BF16, f"QT{g}") for g in range(8)]
        persist_mark = A.off

        invf = smalls.ap[:, 0:1]; rowf = smalls.ap[:, 1:2]; colf = smalls.ap[:, 2:3]; sgn = smalls.ap[:, 3:4]
        neglam = smalls.ap[:, 4:5]; pof = smalls.ap[:, 5:6]

        P.op("gp", lambda e: e.iota(tmp_i.ap[:, 0:128], pattern=[[1, 128]], base=0, channel_multiplier=-1), writes=[tmp_i])
        P.op("dve", ts(ident_f.ap, tmp_i.ap[:, 0:128], 0.0, None, ALU.is_equal), reads=[tmp_i], writes=[ident_f])
        P.op("dve", cp(ident_b.ap, ident_f.ap), reads=[ident_f], writes=[ident_b])
        P.op("dve", ts(ltri_b.ap, tmp_i.ap[:, 0:128], 0.0, None, ALU.is_gt), reads=[tmp_i], writes=[ltri_b])
        P.op("pool", mset(ones_b.ap, 1.0), writes=[ones_b])
        P.op("pool", mset(bones.ap, 0.0), writes=[bones])
        P.op("pool", mset(bones.ap[0:64, 0:64], 1.0), writes=[bones])
        P.op("pool", mset(bones.ap[64:128, 64:128], 1.0), writes=[bones])
        P.op("pool", mset(one_c.ap, 1.0), writes=[one_c])
        P.op("gp", lambda e: e.iota(tmp_i.ap[:, 128:160], pattern=[[1, 32]], base=0, channel_multiplier=0), writes=[tmp_i])
        P.op("dve", cp(iota_e.ap, tmp_i.ap[:, 128:160]), reads=[tmp_i], writes=[iota_e])
        P.dma("sp", dmaf(pm.ap, D["pmeta"]), "c0", writes=[pm])
        P.dma("sp", dmaf(gq_t.ap, D["gq"]), "c1", writes=[gq_t])
        P.dma("sp", dmaf(gk_t.ap, D["gk"]), "c2", writes=[gk_t])
        P.dma("sp", dmaf(smalls.ap[:, 8:9], D["pos0"]), "c3", writes=[smalls])
        P.op("act", actf(invf, pm.ap[:, 0:1], AF.Exp, scale=-math.log(10000.0) / 16.0), reads=[pm], writes=[smalls])
        P.op("dve", tt(colf, invf, pm.ap[:, 1:2], ALU.mult), reads=[smalls, pm], writes=[smalls])
        P.op("dve", tt(rowf, invf, colf, ALU.subtract), reads=[smalls], writes=[smalls])
        P.op("dve", cp(sgn, pm.ap[:, 2:3]), reads=[pm], writes=[smalls])
        P.op("dve", tt(pof, smalls.ap[:, 8:9], rowf, ALU.mult), reads=[smalls], writes=[smalls])
        P.op("gp", lambda e: e.iota(tmp_i.ap, pattern=[[1, 8], [0, 64]], base=0, channel_multiplier=0), writes=[tmp_i])
        P.op("dve", ts(A0.ap, tmp_i.ap, rowf, None, ALU.mult), reads=[tmp_i, smalls], writes=[A0])
        P.op("gp", lambda e: e.iota(tmp_i.ap, pattern=[[0, 8], [1, 64]], base=0, channel_multiplier=0), reads=[A0], writes=[tmp_i])
        P.op("dve", cp(tmp_f.ap, tmp_i.ap), reads=[tmp_i], writes=[tmp_f])
        P.op("dve", stt(A0.ap, tmp_f.ap, colf, A0.ap, ALU.mult, ALU.add), reads=[tmp_f, smalls, A0], writes=[A0])
        P.op("gp", lambda e: e.iota(tmp_i.ap[:, 0:16], pattern=[[8, 16]], base=0, channel_multiplier=0), reads=[tmp_f], writes=[tmp_i])
        P.op("dve", ts(offs.ap, tmp_i.ap[:, 0:16], rowf, None, ALU.mult), reads=[tmp_i, smalls], writes=[offs])
        P.op("dve", ts(offq.ap, offs.ap[:, 0:4], pof, None, ALU.add), reads=[offs, smalls], writes=[offq])

        mark0 = A.off
        cv = A.alloc([128, 16], F32)
        sv = A.alloc([128, 16], F32)
        svb = A.alloc([128, 16, 128], F32)
        wst = [A.alloc([128, 8, 512], F32, f"wst{i}") for i in range(2)]
        bst = [A.alloc([128, 512], F32, f"bst{i}") for i in range(2)]
        gbc = A.alloc([128, 1024], F32)
        P.dma("sp", dmaf(cv.ap, D["cvec"]), "c4", writes=[cv])
        P.op("act", actf(sv.ap, cv.ap, AF.Silu), reads=[cv], writes=[sv])
        P.op("dve", cp(svb.ap, sv.ap.unsqueeze(2).to_broadcast([128, 16, 128])), reads=[sv], writes=[svb])
        w_ada_v = D["w_ada"].rearrange("(k p) n -> p k n", p=128)
        jobs = [(0, [(0, 0, None), (1, 2, None)]), (1, [(0, 1, "g_attn"), (1, 3, "g_attn")]),
                (2, [(0, 4, None)]), (3, [(0, 5, None)]), (4, [(0, 6, "g_ffn")]), (5, [(0, 7, None)])]
        it = 0
        last_g = None
        for chunk, dests in jobs:
            for half in range(2):
                s = it % 2
                it += 1
                c0 = chunk * 1024 + half * 512
                P.dma("sp", dmaf(wst[s].ap, w_ada_v[:, :, c0:c0 + 512]), f"wst{s}", writes=[wst[s]])
                P.dma("sp", dmaf(bst[s].ap, D["b_ada"][c0:c0 + 512].partition_broadcast(128)), f"bst{s}", writes=[bst[s]])
                for (j, di, gname) in dests:
                    if gname is not None and gname != last_g:
                        P.dma("sp", dmaf(gbc.ap, D[gname].partition_broadcast(128)), "gbc", writes=[gbc])
                        last_g = gname
                    pb = PS[(it + j) % 2]
                    P.pe_group([mm(pb.ap, svb.ap[:, k * 2 + j, :], wst[s].ap[:, k, :], k == 0, k == 7) for k in range(8)],
                               reads=[svb, wst[s]], writes=[pb])
                    dst = modbc[di].ap[:, half * 512:(half + 1) * 512]
                    P.op("dve", tt(dst, pb.ap, bst[s].ap, ALU.add), reads=[pb, bst[s]], writes=[modbc[di]])
                    if gname is not None:
                        P.op("dve", stt(dst, dst, 1.0, gbc.ap[:, half * 512:(half + 1) * 512], ALU.add, ALU.mult),
                             reads=[modbc[di], gbc], writes=[modbc[di]])
        if stage == 0:
            toks = []
            for i in range(8):
                toks.append(P.dma("sp", dmaf(dbg["mod"][:, i, :], modbc[i].ap), "dbg", reads=[modbc[i]]))
            P.wait_all("sp", toks)
        P.barrier()
        A.off = mark0

        L = dict(locals())
        if stage >= 1:
            phase1(nc, P, A, PS, psb, D, L)
        if stage == 1:
            P.barrier()
            t1 = P.dma("sp", dmaf(dbg["kT"], kT_scr.ap), "dbg", reads=[kT_scr])
            t2 = P.dma("sp", dmaf(dbg["v"], v_scr.ap), "dbg", reads=[v_scr])
            toks = [t1, t2]
            for g in range(8):
                toks.append(P.dma("sp", dmaf(dbg["qT"][:, g, :], QT[g].ap), "dbg", reads=[QT[g]]))
            P.wait_all("sp", toks)
        P.barrier()
        A.off = persist_mark

        if stage >= 2:
            phase2(nc, P, A, PS, psb, D, L)
        if stage == 2:
            P.barrier()
            P.wait_all("sp", [P.dma("sp", dmaf(dbg["lat"], L["lat"].ap), "dbg", reads=[L["lat"]])])
        P.barrier()

        if stage >= 3:
            phase3(nc, P, A, PS, psb, D, L, dbg, stage, out_d)
        P.barrier()

        P.plan()
        sem_names = list(P.CE) + sorted(P.dcnt.keys())
        with nc.cleanup_on_exit():
            sems = {n: nc.alloc_semaphore("s_" + n.replace(":", "_")) for n in sem_names}
            for h in sems.values():
                nc.gpsimd.sem_clear(h)
            nc.all_engine_barrier()
            with nc.Block() as block:
                @block.tensor
                def _(e):
                    P.emit("pe", e, sems)

                @block.scalar
                def _(e):
                    P.emit("act", e, sems)

                @block.vector
                def _(e):
                    P.emit("dve", e, sems)

                @block.gpsimd
                def _(e):
                    P.emit("pool", e, sems)

                @block.sync
                def _(e):
                    P.emit("sp", e, sems)
    return nc


def phase1(nc, P, A, PS, psb, D, L):
    ident_b = L["ident_b"]; bones = L["bones"]; modbc = L["modbc"]; QT = L["QT"]
    A0 = L["A0"]; offs = L["offs"]; offq = L["offq"]; smalls = L["smalls"]
    gq_t = L["gq_t"]; gk_t = L["gk_t"]; kT_scr = L["kT_scr"]; v_scr = L["v_scr"]
    sgn = L["sgn"]
    xin = [A.alloc([128, 1024], F32, f"xin{i}") for i in range(3)]
    junk = A.alloc([128, 1024], BF16)
    st = [A.alloc([128, 4], F32, f"st{i}") for i in range(3)]
    t1 = [A.alloc([128, 1024], F32, f"t1{i}") for i in range(2)]
    hb = [A.alloc([128, 1024], BF16, f"hb{i}") for i in range(2)]
    hT = [A.alloc([128, 8, 512], BF16, f"hT{i}") for i in range(2)]
    wbuf = A.alloc([128, 8, 2048], BF16)
    Ct = [A.alloc([128, 512], F32, f"Ct{i}") for i in range(2)]
    St = [A.alloc([128, 512], F32, f"St{i}") for i in range(2)]
    ang = A.alloc([128, 512], F32); angi = A.alloc([128, 512], I32); red = A.alloc([128, 512], F32)
    sq = A.alloc([128, 512], F32); kg = A.alloc([128, 512], F32); krg = A.alloc([128, 512], F32)
    ta = [A.alloc([128, 512], F32, f"ta{i}") for i in range(2)]
    tb = [A.alloc([128, 512], F32, f"tb{i}") for i in range(2)]
    rbc = A.alloc([128, 512], F32)
    kout = [[A.alloc([128, 512], BF16, f"ko{g}{i}") for i in range(2)] for g in range(5)]
    vst = [A.alloc([128, 4, 640], BF16, f"vst{i}") for i in range(2)]
    TP = PS[0]; KP = [PS[1], PS[2]]; KRP = [PS[3], PS[4]]; VP = [PS[5], PS[6]]; SSB = PS[7]

    def make_tables(slot, off_ap, off_r):
        for (dst, shift, scl) in ((St[slot], 0.0, sgn), (Ct[slot], math.pi / 2, None)):
            P.op("dve", ts(ang.ap, A0.ap, off_ap, shift, ALU.add, ALU.add), reads=[A0, off_r], writes=[ang])
            P.op("dve", ts(angi.ap, ang.ap, 1.0 / TWO_PI, None, ALU.mult), reads=[ang], writes=[angi])
            P.op("dve", stt(red.ap, angi.ap, -TWO_PI, ang.ap, ALU.mult, ALU.add), reads=[angi, ang], writes=[red])
            P.op("dve", ts(red.ap, red.ap, 3.14159, -3.14159, ALU.min, ALU.max), reads=[red], writes=[red])
            if scl is None:
                P.op("act", actf(dst.ap, red.ap, AF.Sin), reads=[red], writes=[dst])
            else:
                P.op("act", actf(dst.ap, red.ap, AF.Sin, scale=scl), reads=[red, smalls], writes=[dst])

    def ident_tables(slot):
        P.op("pool", mset(Ct[slot].ap, 1.0), writes=[Ct[slot]])
        P.op("pool", mset(St[slot].ap, 0.0), writes=[St[slot]])

    state = {"xi": 0, "ti": 0, "gi": 0, "ko": 0}

    def token_tile_a(src_ap, sh_r, gm_r):
        i = state["xi"]; state["xi"] += 1
        xs = xin[i % 3]; s_ = st[i % 3]; t_ = t1[i % 2]; h_ = hb[i % 2]
        P.dma("sp", dmaf(xs.ap, src_ap), f"xin{i % 3}", writes=[xs])
        P.op("act", actf(junk.ap, xs.ap, AF.Square, accum=s_.ap[:, 0:1]), reads=[xs], writes=[junk, s_])
        P.op("act", actf(s_.ap[:, 1:2], s_.ap[:, 0:1], AF.Ln, bias=EPS, scale=1.0 / 1024.0), reads=[s_], writes=[s_])
        P.op("act", actf(s_.ap[:, 2:3], s_.ap[:, 1:2], AF.Exp, scale=-0.5), reads=[s_], writes=[s_])
        P.op("dve", stt(t_.ap, xs.ap, s_.ap[:, 2:3], gm_r.ap, ALU.mult, ALU.mult), reads=[xs, s_, gm_r], writes=[t_])
        P.op("pool", tt(h_.ap, t_.ap, sh_r.ap, ALU.add), reads=[t_, sh_r], writes=[h_])
        return h_

    def token_tile_b(h_, hT_r, col0):
        P.pe_group([trp(psb(0)[:, k * 128:(k + 1) * 128], h_.ap[:, k * 128:(k + 1) * 128], ident_b.ap) for k in range(8)],
                   reads=[h_, ident_b], writes=[TP])
        P.op("act", cp_act(hT_r.ap[:, :, col0:col0 + 128], psb(0).rearrange("p (k n) -> p k n", k=8)),
             reads=[TP], writes=[hT_r])

    def token_tile(src_ap, sh_r, gm_r, hT_r, col0):
        token_tile_b(token_tile_a(src_ap, sh_r, gm_r), hT_r, col0)

    def cp_act(out, in_):
        return lambda e: e.activation(out=out, in_=in_, func=AF.Copy)

    def qk_features(hT_r, ntok, ngroups, wcol0, rotcol0, g_t, norm_groups, tslot, dest_fn, between=(), vtiles=()):
        held = {}
        for g in range(ngroups):
            j = state["gi"] % 2; state["gi"] += 1
            kp, krp = KP[j], KRP[j]
            if g < len(between):
                held[g] = between[g][0]()
            P.pe_group([mm(kp.ap[:, 0:ntok], wbuf.ap[:, k, wcol0 + g * 128: wcol0 + (g + 1) * 128], hT_r.ap[:, k, 0:ntok], k == 0, k == 7)
                        for k in range(8)], reads=[wbuf, hT_r], writes=[kp])
            P.pe_group([mm(krp.ap[:, 0:ntok], wbuf.ap[:, k, rotcol0 + g * 128: rotcol0 + (g + 1) * 128], hT_r.ap[:, k, 0:ntok], k == 0, k == 7)
                        for k in range(8)], reads=[wbuf, hT_r], writes=[krp])
            if (g - 1) in held:
                between[g - 1][1](held.pop(g - 1))
            if g == ngroups - 1 and g in held:
                between[g][1](held.pop(g))
            if g < len(vtiles):
                vtiles[g]()
            dst_r, dst_ap, after = dest_fn(g)
            ta_, tb_ = ta[j], tb[j]
            C_, S_ = Ct[tslot], St[tslot]
            if g in norm_groups:
                P.op("act", actf(sq.ap[:, 0:ntok], kp.ap[:, 0:ntok], AF.Square), reads=[kp], writes=[sq])
                P.pe_group([mm(SSB.ap[:, 0:ntok], bones.ap, sq.ap[:, 0:ntok], True, True)], reads=[bones, sq], writes=[SSB])
                P.op("act", actf(rbc.ap[:, 0:ntok], SSB.ap[:, 0:ntok], AF.Ln, bias=EPS, scale=1.0 / 64.0), reads=[SSB], writes=[rbc])
                P.op("act", actf(rbc.ap[:, 0:ntok], rbc.ap[:, 0:ntok], AF.Exp, scale=-0.5), reads=[rbc], writes=[rbc])
                P.op("act", actf(kg.ap[:, 0:ntok], kp.ap[:, 0:ntok], AF.Copy, scale=g_t.ap[:, 0:1]), reads=[kp, g_t], writes=[kg])
                P.op("act", actf(krg.ap[:, 0:ntok], krp.ap[:, 0:ntok], AF.Copy, scale=g_t.ap[:, 1:2]), reads=[krp, g_t], writes=[krg])
                P.op("dve", tt(ta_.ap[:, 0:ntok], kg.ap[:, 0:ntok], C_.ap[:, 0:ntok], ALU.mult), reads=[kg, C_], writes=[ta_])
                P.op("pool", tt(tb_.ap[:, 0:ntok], krg.ap[:, 0:ntok], S_.ap[:, 0:ntok], ALU.mult), reads=[krg, S_], writes=[tb_])
                P.op("dve", tt(ta_.ap[:, 0:ntok], ta_.ap[:, 0:ntok], tb_.ap[:, 0:ntok], ALU.add), reads=[ta_, tb_], writes=[ta_])
                P.op("dve", tt(dst_ap, ta_.ap[:, 0:ntok], rbc.ap[:, 0:ntok], ALU.mult), reads=[ta_, rbc], writes=[dst_r])
            else:
                P.op("dve", tt(ta_.ap[:, 0:ntok], kp.ap[:, 0:ntok], C_.ap[:, 0:ntok], ALU.mult), reads=[kp, C_], writes=[ta_])
                P.op("dve", tt(tb_.ap[:, 0:ntok], krp.ap[:, 0:ntok], S_.ap[:, 0:ntok], ALU.mult), reads=[krp, S_], writes=[tb_])
                P.op("pool", tt(dst_ap, ta_.ap[:, 0:ntok], tb_.ap[:, 0:ntok], ALU.add), reads=[ta_, tb_], writes=[dst_r])
            if after is not None:
                after()

    wkv_v = D["wkv"].rearrange("(k p) n -> p k n", p=128)
    for hlf in range(2):
        P.dma("pool", dmaf(wbuf.ap[:, hlf * 4:(hlf + 1) * 4, 0:1920], wkv_v[:, hlf * 4:(hlf + 1) * 4, :]), "wbuf", writes=[wbuf])
    store_toks = []
    ngrp = 17
    work = []
    for grp in range(ngrp):
        ntok = 256 if grp == 0 else 512
        tok0 = 0 if grp == 0 else 256 + (grp - 1) * 512
        work.append(dict(kind="kv", grp=grp, ntok=ntok, tok0=tok0, src=D["xkv"], off=(None if grp == 0 else (offs, grp - 1)),
                         mods=(modbc[2], modbc[3]) if grp == 0 else (modbc[0], modbc[1])))
    for qg in range(4):
        work.append(dict(kind="q", grp=ngrp + qg, qg=qg, ntok=512, tok0=qg * 512, src=D["xq"], off=(L["offq"], qg), mods=(modbc[0], modbc[1])))

    def prep_tables(w):
        tslot = w["grp"] % 2
        if w["off"] is None:
            ident_tables(tslot)
        else:
            r_, c_ = w["off"]
            make_tables(tslot, r_.ap[:, c_:c_ + 1], r_)

    def tile_fn(w, t):
        def fa():
            return token_tile_a(w["src"][w["tok0"] + t * 128: w["tok0"] + (t + 1) * 128, :], w["mods"][0], w["mods"][1])

        def fb(h_):
            token_tile_b(h_, hT[w["grp"] % 2], t * 128)
        return (fa, fb)

    prep_tables(work[0])
    for t in range(work[0]["ntok"] // 128):
        fa, fb = tile_fn(work[0], t)
        fb(fa())
    wq_v = D["wq"].rearrange("(k p) n -> p k n", p=128)
    for wi, w in enumerate(work):
        grp, ntok, tok0 = w["grp"], w["ntok"], w["tok0"]
        hT_r = hT[grp % 2]
        tslot = grp % 2
        between = []
        if wi + 1 < len(work):
            nw = work[wi + 1]
            prep_tables(nw)
            between = [tile_fn(nw, t) for t in range(nw["ntok"] // 128)]
        if w["kind"] == "kv":
            def dest_fn(g, grp=grp, ntok=ntok, tok0=tok0):
                ko = kout[g][grp % 2]

                def after():
                    store_toks.append(P.dma("pool", dmaf(kT_scr.ap[g, :, tok0:tok0 + ntok], ko.ap[:, 0:ntok]), f"ko{g}{grp % 2}",
                                            reads=[ko], writes=[]))
                return ko, ko.ap[:, 0:ntok], after

            vs_ = vst[grp % 2]

            def vtile(t, hT_r=hT_r, vs_=vs_):
                def f():
                    P.pe_group([mm(VP[0].ap, hT_r.ap[:, k, t * 128:(t + 1) * 128], wbuf.ap[:, k, 1280:1792], k == 0, k == 7) for k in range(8)] +
                               [mm(VP[1].ap[:, 0:128], hT_r.ap[:, k, t * 128:(t + 1) * 128], wbuf.ap[:, k, 1792:1920], k == 0, k == 7) for k in range(8)],
                               reads=[hT_r, wbuf], writes=[VP[0], VP[1]])
                    P.op("act", cp_act(vs_.ap[:, t, 0:512], VP[0].ap), reads=[VP[0]], writes=[vs_])
                    P.op("dve", cp(vs_.ap[:, t, 512:640], VP[1].ap[:, 0:128]), reads=[VP[1]], writes=[vs_])
                return f

            qk_features(hT_r, ntok, 5, 0, 640, gk_t, (0,), tslot, dest_fn, between=between[:4],
                        vtiles=[vtile(t) for t in range(ntok // 128)])
            nt = ntok // 128
            store_toks.append(P.dma("pool", dmaf(v_scr.ap[tok0:tok0 + ntok, :].rearrange("(t p) n -> p t n", p=128), vs_.ap[:, 0:nt, :]),
                                    f"vst{grp % 2}", reads=[vs_], writes=[]))
            if grp == ngrp - 1:
                for hlf in range(2):
                    P.dma("pool", dmaf(wbuf.ap[:, hlf * 4:(hlf + 1) * 4, :], wq_v[:, hlf * 4:(hlf + 1) * 4, :]), "wbuf", writes=[wbuf])
        else:
            qg = w["qg"]

            def dest_fn(g, qg=qg):
                return QT[g], QT[g].ap[:, qg * 512:(qg + 1) * 512], None

            qk_features(hT_r, 512, 8, 0, 1024, L["gq_t"], (0, 1, 2, 3), tslot, dest_fn, between=between)
    L["store_toks"] = store_toks


def phase2(nc, P, A, PS, psb, D, L):
    QT = L["QT"]; kT_scr = L["kT_scr"]; v_scr = L["v_scr"]; ps_t = L["ps_t"]; smalls = L["smalls"]
    neglam = L["neglam"]
    lat = A.alloc([128, 16, 1024], BF16, "lat")
    L["lat"] = lat
    L["lat_end"] = A.off
    KT = [A.alloc([128, NK], BF16, f"KT{i}") for i in range(2)]
    VV = [A.alloc([128, NKT, 132], BF16, f"VV{i}") for i in range(2)]
    PT = [A.alloc([128, 1024], BF16, f"PT{i}") for i in range(3)]
    rec = [A.alloc([128, 4], F32, f"rec{i}") for i in range(2)]
    o1 = A.alloc([128, 4, 128], F32); o2 = A.alloc([128, 4, 128], F32)
    dd = A.alloc([128, 4, 128], F32); sqd = A.alloc([128, 4, 128], F32)
    ssd = A.alloc([128, 8], F32)
    subbc = A.alloc([128, 128], F32)
    dl = A.alloc([128, 256], F32); dl2 = A.alloc([128, 256], F32); ls = A.alloc([128, 4], F32)
    zt2 = A.alloc([128, 1024], F32, "zt2")
    S = [[PS[0], PS[1]], [PS[2], PS[3]]]
    Oset = [R(None, "O0"), R(None, "O1")]

    def Ov(m):
        return ps_t[:, 4 + 2 * m:6 + 2 * m, :].rearrange("p b (j c) -> p (b j) c", j=2)

    P.dma("sp", dmaf(dl.ap, D["dlam"].partition_broadcast(128)), "dl", writes=[dl])
    P.dma("sp", dmaf(subbc.ap, D["subln"].partition_broadcast(128)), "sub", writes=[subbc])
    P.op("dve", ts(subbc.ap, subbc.ap, 0.8, None, ALU.mult), reads=[subbc], writes=[subbc])
    P.op("dve", tt(dl2.ap[:, 0:64], dl.ap[:, 0:64], dl.ap[:, 64:128], ALU.mult), reads=[dl], writes=[dl2])
    P.op("dve", tt(dl2.ap[:, 64:128], dl.ap[:, 128:192], dl.ap[:, 192:256], ALU.mult), reads=[dl], writes=[dl2])
    P.op("dve", lambda e: e.reduce_sum(out=ls.ap[:, 0:2], in_=dl2.ap[:, 0:128].rearrange("p (a b) -> p a b", a=2), axis=AX.X),
         reads=[dl2], writes=[ls])
    P.op("act", actf(ls.ap[:, 2:4], ls.ap[:, 0:2], AF.Exp), reads=[ls], writes=[ls])
    P.op("dve", tt(neglam, ls.ap[:, 3:4], ls.ap[:, 2:3], ALU.subtract), reads=[ls], writes=[smalls])
    P.op("dve", ts(neglam, neglam, -0.2, None, ALU.add), reads=[smalls], writes=[smalls])
    for s_ in range(2):
        P.op("pool", mset(VV[s_].ap[:, :, 0:1], 1.0), writes=[VV[s_]])

    groups = [("A", 0), ("A", 1), ("B", 0), ("B", 1), ("B", 2), ("B", 3)]

    def load_group(gi):
        kind, j = groups[gi]
        slot = gi % 2
        if kind == "A":
            for half in range(2):
                P.dma("sp", dmaf(KT[slot].ap[half * 64:(half + 1) * 64, :], kT_scr.ap[0, j * 64:(j + 1) * 64, :]), f"KT{slot}",
                      reads=[kT_scr], writes=[KT[slot]])
            c0, dv = j * 64, 64
        else:
            P.dma("sp", dmaf(KT[slot].ap, kT_scr.ap[1 + j]), f"KT{slot}", reads=[kT_scr], writes=[KT[slot]])
            c0, dv = 128 + j * 128, 128
        vv = v_scr.ap[:, c0:c0 + dv].rearrange("(t p) n -> p t n", p=128)
        for part in range(6):
            P.dma("sp", dmaf(VV[slot].ap[:, part * 11:(part + 1) * 11, 1:1 + dv], vv[:, part * 11:(part + 1) * 11, :]), f"VV{slot}",
                  reads=[v_scr], writes=[VV[slot]])

    iters = []
    for gi, (kind, j) in enumerate(groups):
        for qt in range(4):
            if kind == "A":
                for pr in range(2):
                    iters.append(dict(gi=gi, qt=qt, kind="A", fg=2 * j + pr, dv=64, hq0=4 * j + 2 * pr))
            else:
                iters.append(dict(gi=gi, qt=qt, kind="B", fg=4 + j, dv=128, h=j))
    steps = [(ii, kt) for ii in range(len(iters)) for kt in range(NKT)]

    def emit_qk(i):
        ii, kt = steps[i]
        itd = iters[ii]
        slot = itd["gi"] % 2
        p = i % 2
        qs_ = slice(itd["qt"] * 512, (itd["qt"] + 1) * 512)
        P.pe_group([mm(S[p][m].ap, KT[slot].ap[m * 64:(m + 1) * 64, kt * 128:(kt + 1) * 128],
                       QT[itd["fg"]].ap[m * 64:(m + 1) * 64, qs_], True, True) for m in range(2)],
                   reads=[KT[slot], QT[itd["fg"]]], writes=[S[p][0], S[p][1]])

    def emit_exp(i):
        p = i % 2
        sv = ps_t[:, 2 * p:2 * p + 2, :].rearrange("p b c -> p (b c)")
        P.op("act", actf(PT[i % 3].ap, sv, AF.Exp, scale=0.125), reads=[S[p][0], S[p][1]], writes=[PT[i % 3]])

    def emit_pv(i):
        ii, kt = steps[i]
        itd = iters[ii]
        slot = itd["gi"] % 2
        dv = itd["dv"]
        fns = []
        for m in range(2):
            ov = Ov(m)
            pt = PT[i % 3]
            for j in range(4):
                kw = {"skip_group_check": True} if j % 2 == 1 else {}
                fns.append(mm(ov[:, j, 0:dv + 1], pt.ap[:, m * 512 + j * 128:m * 512 + (j + 1) * 128], VV[slot].ap[:, kt, 0:dv + 1],
                              (kt == 0 and j % 2 == 0), kt == NKT - 1, **kw))
        P.pe_group(fns, reads=[PT[i % 3], VV[slot]], writes=[Oset[0], Oset[1]])
        if kt == NKT - 1:
            evac(ii)

    def evac(ii):
        itd = iters[ii]
        qt = itd["qt"]
        dv = itd["dv"]
        for m in range(2):
            ov = Ov(m)
            rc = rec[m]
            P.op("dve", lambda e, rc=rc, ov=ov: e.reciprocal(out=rc.ap.unsqueeze(2), in_=ov[:, :, 0:1]), reads=[Oset[m]], writes=[rc])
            rbc_ = rc.ap.unsqueeze(2).to_broadcast([128, 4, dv])
            if itd["kind"] == "A":
                hq = itd["hq0"] + m
                P.op("dve", tt(lat.ap[:, qt * 4:(qt + 1) * 4, hq * 64:(hq + 1) * 64], ov[:, :, 1:65], rbc_, ALU.mult),
                     reads=[Oset[m], rc], writes=[lat])
            else:
                om = o1 if m == 0 else o2
                P.op("dve", tt(om.ap, ov[:, :, 1:129], rbc_, ALU.mult), reads=[Oset[m], rc], writes=[om])
        if itd["kind"] == "A":
            return
        h = itd["h"]
        P.op("dve", stt(dd.ap, o2.ap, neglam, o1.ap, ALU.mult, ALU.add), reads=[o2, o1, smalls], writes=[dd])
        P.op("pool", tt(sqd.ap, dd.ap, dd.ap, ALU.mult), reads=[dd], writes=[sqd])
        P.op("dve", lambda e: e.reduce_sum(out=ssd.ap[:, 0:4], in_=sqd.ap, axis=AX.X), reads=[sqd], writes=[ssd])
        P.op("act", actf(ssd.ap[:, 4:8], ssd.ap[:, 0:4], AF.Ln, bias=EPS, scale=1.0 / 128.0), reads=[ssd], writes=[ssd])
        P.op("act", actf(ssd.ap[:, 4:8], ssd.ap[:, 4:8], AF.Exp, scale=-0.5), reads=[ssd], writes=[ssd])
        P.op("dve", tt(dd.ap, dd.ap, ssd.ap[:, 4:8].unsqueeze(2).to_broadcast([128, 4, 128]), ALU.mult), reads=[dd, ssd], writes=[dd])
        P.op("dve", tt(lat.ap[:, qt * 4:(qt + 1) * 4, 512 + h * 128:512 + (h + 1) * 128], dd.ap,
                       subbc.ap.unsqueeze(1).to_broadcast([128, 4, 128]), ALU.mult), reads=[dd, subbc], writes=[lat])

    load_group(0)
    load_group(1)
    xs_scr = L["xs_scr"]; ys_scr = L["ys_scr"]
    P.op("pool", mset(zt2.ap, 0.0), writes=[zt2])
    P.dma("sp", dmaf(ys_scr.ap[TRASH:TRASH + 128, :], zt2.ap), "zt", reads=[zt2], writes=[ys_scr])
    ztb = zt2.ap.bitcast(BF16)
    nfull = (TRASH + 128) // 256
    for r in range(nfull):
        P.dma("sp", dmaf(xs_scr.ap[r * 256:(r + 1) * 256, :].rearrange("(p a) d -> p (a d)", a=2), ztb), "zf", reads=[zt2], writes=[xs_scr])
    rem = (TRASH + 128) - nfull * 256
    if rem:
        P.dma("sp", dmaf(xs_scr.ap[nfull * 256:nfull * 256 + rem, :], ztb[0:rem, 0:1024]), "zf", reads=[zt2], writes=[xs_scr])
    n = len(steps)
    emit_qk(0)
    for i in range(n):
        ii, kt = steps[i]
        if kt == 0 and ii > 0 and iters[ii]["gi"] != iters[ii - 1]["gi"]:
            gnext = iters[ii]["gi"] + 1
            if gnext < len(groups):
                load_group(gnext)
        if i + 1 < n:
            emit_qk(i + 1)
        emit_exp(i)
        emit_pv(i)


def phase3(nc, P, A, PS, psb, D, L, dbg, stage, out_d):
    ident_b = L["ident_b"]; ident_f = L["ident_f"]; modbc = L["modbc"]; lat = L["lat"]; ones_b = L["ones_b"]
    ltri_b = L["ltri_b"]; iota_e = L["iota_e"]; xs_scr = L["xs_scr"]; ys_scr = L["ys_scr"]
    arena_t = L["arena_t"]
    x1_scr = R(nc.dram_tensor("x1_scr", [NQ, 1024], F32, kind="Internal").ap())
    A2 = Arena(arena_t, L["persist_mark"])
    A2.off = L["late_mark"]
    A.off = L["lat_end"]
    rowi = A2.alloc([128, 16, 4], I32, "rowi")
    gkk = A2.alloc([128, 16, 4], F32, "gkk")
    base = A2.alloc([128, 32], F32, "base")
    wr = A2.alloc([128, 8, 32], F32, "wr")
    brbc = A2.alloc([128, 32], F32, "brbc")
    zt = A2.alloc([128, 1024], F32, "zt")
    sm = [A2.alloc([128, 64], F32, f"sm{i}") for i in range(2)]
    smi = [A2.alloc([128, 8], U32, f"smi{i}") for i in range(2)]
    a2_mark = A2.off
    mark3 = A.off
    wo = A.alloc([128, 8, 1024], BF16, "wo")
    lmT = [A.alloc([128, 8, 128], BF16, f"lmT{i}") for i in range(2)]
    xt = [A.alloc([128, 1024], F32, f"xt{i}") for i in range(2)]
    tmp = [A.alloc([128, 1024], F32, f"tmp{i}") for i in range(2)]
    x1t = [A.alloc([128, 1024], F32, f"x1t{i}") for i in range(2)]
    h2f = [A.alloc([128, 1024], F32, f"h2f{i}") for i in range(2)]
    h2b = [A.alloc([128, 1024], BF16, f"h2b{i}") for i in range(2)]
    junk = A.alloc([128, 1024], BF16, "junk3")
    h2T = A2.alloc([128, 8, 128], F32, "h2T")
    lg = [A2.alloc([128, 32], F32, f"lg{i}") for i in range(2)]
    oh = A2.alloc([128, 32], F32, "oh")
    pos = [A2.alloc([128, 32], F32, f"pos{i}") for i in range(2)]
    maskb = [A2.alloc([128, 32], BF16, f"maskb{i}") for i in range(2)]
    TP = PS[0]; PO = [PS[1], PS[2]]; HT = [PS[3], PS[4]]; LG = PS[5]; PP = PS[6]; PC = PS[7]

    P.dma("pool", dmaf(wo.ap, D["w_o"].rearrange("(k p) n -> p k n", p=128)), "wo", writes=[wo])
    P.dma("sp", dmaf(wr.ap, D["w_r"].rearrange("(k p) n -> p k n", p=128)), "wr", writes=[wr])
    P.dma("sp", dmaf(brbc.ap, D["b_r"].partition_broadcast(128)), "brbc", writes=[brbc])
    P.op("pool", mset(base.ap, 0.0), writes=[base])
    P.op("pool", mset(zt.ap, 0.0), writes=[zt])
    x1_toks = []
    for i in range(16):
        s_ = i % 2
        P.dma("sp", dmaf(xt[s_].ap, D["xq"][i * 128:(i + 1) * 128, :]), f"xt{s_}", writes=[xt[s_]])
        P.pe_group([trp(psb(0)[:, k * 128:(k + 1) * 128], lat.ap[:, i, k * 128:(k + 1) * 128], ident_b.ap) for k in range(8)],
                   reads=[lat, ident_b], writes=[TP])
        P.op("act", lambda e, o=lmT[s_].ap: e.activation(out=o, in_=psb(0).rearrange("p (k n) -> p k n", k=8), func=AF.Copy),
             reads=[TP], writes=[lmT[s_]])
        for h in range(2):
            P.pe_group([mm(PO[h].ap, lmT[s_].ap[:, k, :], wo.ap[:, k, h * 512:(h + 1) * 512], k == 0, k == 7) for k in range(8)],
                       reads=[lmT[s_], wo], writes=[PO[h]])
            P.op("dve", tt(tmp[s_].ap[:, h * 512:(h + 1) * 512], PO[h].ap, modbc[4].ap[:, h * 512:(h + 1) * 512], ALU.mult),
                 reads=[PO[h], modbc[4]], writes=[tmp[s_]])
        P.op("dve", tt(x1t[s_].ap, tmp[s_].ap, xt[s_].ap, ALU.add), reads=[tmp[s_], xt[s_]], writes=[x1t[s_]])
        x1_toks.append(P.dma("pool", dmaf(x1_scr.ap[i * 128:(i + 1) * 128, :], x1t[s_].ap), f"x1s{s_}", reads=[x1t[s_]]))
        st_ = sm[s_]
        P.op("act", actf(junk.ap, x1t[s_].ap, AF.Square, accum=st_.ap[:, 0:1]), reads=[x1t[s_]], writes=[junk, st_])
        P.op("act", actf(st_.ap[:, 1:2], st_.ap[:, 0:1], AF.Ln, bias=EPS, scale=1.0 / 1024.0), reads=[st_], writes=[st_])
        P.op("act", actf(st_.ap[:, 2:3], st_.ap[:, 1:2], AF.Exp, scale=-0.5), reads=[st_], writes=[st_])
        P.op("dve", stt(tmp[s_].ap, x1t[s_].ap, st_.ap[:, 2:3], modbc[6].ap, ALU.mult, ALU.mult), reads=[x1t[s_], st_, modbc[6]], writes=[tmp[s_]])
        P.op("dve", tt(h2f[s_].ap, tmp[s_].ap, modbc[5].ap, ALU.add), reads=[tmp[s_], modbc[5]], writes=[h2f[s_]])
        P.op("act", lambda e, o=h2b[s_].ap, a=h2f[s_].ap: e.activation(out=o, in_=a, func=AF.Copy), reads=[h2f[s_]], writes=[h2b[s_]])
        for hh in range(2):
            P.pe_group([trp(HT[hh].ap[:, k * 128:(k + 1) * 128], h2f[s_].ap[:, (hh * 4 + k) * 128:(hh * 4 + k + 1) * 128], ident_f.ap)
                        for k in range(4)], reads=[h2f[s_], ident_f], writes=[HT[hh]])
        P.op("act", lambda e: e.activation(out=h2T.ap[:, 0:4, :], in_=HT[0].ap.rearrange("p (k n) -> p k n", k=4), func=AF.Copy),
             reads=[HT[0]], writes=[h2T])
        P.op("dve", cp(h2T.ap[:, 4:8, :], HT[1].ap.rearrange("p (k n) -> p k n", k=4)), reads=[HT[1]], writes=[h2T])
        P.pe_group([mm(LG.ap[:, 0:32], h2T.ap[:, k, :], wr.ap[:, k, :], k == 0, k == 7) for k in range(8)], reads=[h2T, wr], writes=[LG])
        lg_ = lg[s_]; mi_ = smi[s_]
        P.op("dve", tt(lg_.ap, LG.ap[:, 0:32], brbc.ap, ALU.add), reads=[LG, brbc], writes=[lg_])
        mx8 = st_.ap[:, 8:16]
        P.op("dve", lambda e, o=mx8, a=lg_.ap: e.max(out=o, in_=a), reads=[lg_], writes=[st_])
        P.op("dve", lambda e, o=mi_.ap, m=mx8, a=lg_.ap: e.max_index(out=o, in_max=m, in_values=a), reads=[lg_, st_], writes=[mi_])
        P.op("dve", ts(maskb[s_].ap, lg_.ap, st_.ap[:, 11:12], None, ALU.is_ge), reads=[lg_, st_], writes=[maskb[s_]])
        P.op("dve", ts(st_.ap[:, 16:17], st_.ap[:, 8:9], -1.0, None, ALU.mult), reads=[st_], writes=[st_])
        P.op("act", actf(st_.ap[:, 20:24], st_.ap[:, 8:12], AF.Exp, bias=st_.ap[:, 16:17]), reads=[st_], writes=[st_])
        P.op("dve", lambda e, o=st_.ap[:, 24:25], a=st_.ap[:, 20:24]: e.reduce_sum(out=o, in_=a, axis=AX.X), reads=[st_], writes=[st_])
        P.op("dve", lambda e, o=st_.ap[:, 25:26], a=st_.ap[:, 24:25]: e.reciprocal(out=o, in_=a), reads=[st_], writes=[st_])
        P.op("dve", ts(gkk.ap[:, i, :], st_.ap[:, 20:24], st_.ap[:, 25:26], None, ALU.mult), reads=[st_], writes=[gkk])
        P.pe_group([mm(PP.ap[:, 0:32], ltri_b.ap, maskb[s_].ap, True, True)], reads=[ltri_b, maskb[s_]], writes=[PP])
        P.pe_group([mm(PC.ap[:, 0:32], ones_b.ap, maskb[s_].ap, True, True)], reads=[ones_b, maskb[s_]], writes=[PC])
        pos_ = pos[s_]
        P.op("dve", tt(pos_.ap, PP.ap[:, 0:32], base.ap, ALU.add), reads=[PP, base], writes=[pos_])
        P.op("dve", tt(base.ap, PC.ap[:, 0:32], base.ap, ALU.add), reads=[PC, base], writes=[base])
        P.op("dve", cp(st_.ap[:, 28:32], mi_.ap[:, 0:4]), reads=[mi_], writes=[st_])
        for k in range(4):
            P.op("dve", ts(oh.ap, iota_e.ap, st_.ap[:, 28 + k:29 + k], None, ALU.is_equal), reads=[iota_e, st_], writes=[oh])
            P.op("dve", tt(oh.ap, oh.ap, pos_.ap, ALU.mult), reads=[oh, pos_], writes=[oh])
            P.op("dve", lambda e, k=k, st_=st_: e.reduce_sum(out=st_.ap[:, 32 + k:33 + k], in_=oh.ap, axis=AX.X), reads=[oh], writes=[st_])
        P.op("dve", stt(st_.ap[:, 36:40], st_.ap[:, 28:32], float(CAP), st_.ap[:, 32:36], ALU.mult, ALU.add), reads=[st_], writes=[st_])
        P.op("dve", ts(st_.ap[:, 40:44], st_.ap[:, 32:36], float(CAP), None, ALU.is_ge), reads=[st_], writes=[st_])
        P.op("dve", ts(st_.ap[:, 44:48], st_.ap[:, 36:40], -1.0, float(TRASH), ALU.mult, ALU.add), reads=[st_], writes=[st_])
        P.op("dve", tt(st_.ap[:, 44:48], st_.ap[:, 44:48], st_.ap[:, 40:44], ALU.mult), reads=[st_], writes=[st_])
        P.op("dve", tt(st_.ap[:, 36:40], st_.ap[:, 36:40], st_.ap[:, 44:48], ALU.add), reads=[st_], writes=[st_])
        P.op("dve", cp(rowi.ap[:, i, :], st_.ap[:, 36:40]), reads=[st_], writes=[rowi])
        for k in range(4 if stage != 3 else 0):
            P.dma("pool", lambda e, i=i, k=k, s_=s_: e.indirect_dma_start(
                out=xs_scr.ap, out_offset=bass.IndirectOffsetOnAxis(ap=rowi.ap[:, i, k:k + 1], axis=0),
                in_=h2b[s_].ap, in_offset=None), f"sc{s_}", reads=[rowi, h2b[s_]], writes=[xs_scr])
    if stage == 3:
        P.barrier()
        P.wait_all("sp", [P.dma("sp", dmaf(dbg["x1"], x1_scr.ap.rearrange("(t p) n -> p t n", p=128)), "dbg"),
                          P.dma("sp", dmaf(dbg["rowi"], rowi.ap), "dbg"), P.dma("sp", dmaf(dbg["gk"], gkk.ap), "dbg")])
        return
    P.barrier()
    mark3 = a2_mark
    A.off = mark3

    win = [A.alloc([128, 8, 2048], BF16, f"win{i}") for i in range(2)]
    wout = [A.alloc([128, 8, 1024], BF16, f"wout{i}") for i in range(2)]
    xs = [A.alloc([128, 3, 1024], BF16, f"xs{i}") for i in range(2)]
    xsT = [A.alloc([128, 8, SUB], BF16, f"xsT{i}") for i in range(2)]
    actT = A.alloc([128, 8, SUB], BF16, "actT")
    gg = [A.alloc([128, SUB], F32, f"gg{i}") for i in range(2)]
    sg = [A.alloc([128, SUB], F32, f"sg{i}") for i in range(2)]
    ll = [A.alloc([128, SUB], F32, f"ll{i}") for i in range(2)]
    ysst = A.alloc([128, 3, 1024], F32, "ysst")
    binb = [A.alloc([128, 16], F32, f"binb{i}") for i in range(2)]
    boutbc = [A.alloc([128, 1024], F32, f"boutbc{i}") for i in range(2)]
    PG = [PS[1], PS[2]]; PL = [PS[3], PS[4]]; PY = [PS[5], PS[6]]
    w_in_v = D["w_in"].rearrange("e (k p) n -> e p k n", p=128)
    w_out_v = D["w_out"].rearrange("e (k p) n -> e p k n", p=128)
    nslot = SUB // 128

    def load_w(e):
        s_ = e % 2
        for hlf in range(2):
            P.dma("pool", dmaf(win[s_].ap[:, hlf * 4:(hlf + 1) * 4, :], w_in_v[e][:, hlf * 4:(hlf + 1) * 4, :]), f"win{s_}", writes=[win[s_]])
        P.dma("pool", dmaf(wout[s_].ap, w_out_v[e]), f"wout{s_}", writes=[wout[s_]])
        P.dma("sp", dmaf(binb[s_].ap, D["b_in"][e]), f"binb{s_}", writes=[binb[s_]])
        P.dma("sp", dmaf(boutbc[s_].ap, D["b_out"][e].partition_broadcast(128)), f"boutbc{s_}", writes=[boutbc[s_]])

    load_w(0)
    passes = [(e_, sb_) for e_ in range(NE) for sb_ in range(CAP // SUB)]
    TPs = [(PS[0], 0), (PS[7], 7)]
    tpc = [0]

    def load_xs(pi):
        e_, sb_ = passes[pi]
        r0_ = e_ * CAP + sb_ * SUB
        P.dma("sp", dmaf(xs[pi % 2].ap[:, 0:nslot, :], xs_scr.ap[r0_:r0_ + SUB, :].rearrange("(s p) d -> p s d", p=128)), f"xs{pi % 2}",
              reads=[xs_scr], writes=[xs[pi % 2]])

    def emit_xsT(pi):
        s__ = pi % 2
        for sl in range(nslot):
            tpr, tpi = TPs[tpc[0] % 2]
            tpc[0] += 1
            P.pe_group([trp(psb(tpi)[:, k * 128:(k + 1) * 128], xs[s__].ap[:, sl, k * 128:(k + 1) * 128], ident_b.ap) for k in range(8)],
                       reads=[xs[s__], ident_b], writes=[tpr])
            P.op("act", lambda e_, o=xsT[s__].ap[:, :, sl * 128:(sl + 1) * 128], tpi=tpi: e_.activation(
                out=o, in_=psb(tpi).rearrange("p (k n) -> p k n", k=8), func=AF.Copy), reads=[tpr], writes=[xsT[s__]])

    load_xs(0)
    load_xs(1)
    emit_xsT(0)
    for pi, (e, sub) in enumerate(passes):
        ws_ = e % 2
        s_ = pi % 2
        r0 = e * CAP + sub * SUB
        if sub == 0 and e + 1 < NE:
            load_w(e + 1)
        for j in range(8):
            b_ = j % 2
            P.pe_group([mm(PG[b_].ap[:, 0:SUB], win[ws_].ap[:, k, j * 128:(j + 1) * 128], xsT[s_].ap[:, k, :], k == 0, k == 7) for k in range(8)],
                       reads=[win[ws_], xsT[s_]], writes=[PG[b_]])
            P.pe_group([mm(PL[b_].ap[:, 0:SUB], win[ws_].ap[:, k, 1024 + j * 128:1024 + (j + 1) * 128], xsT[s_].ap[:, k, :], k == 0, k == 7)
                        for k in range(8)], reads=[win[ws_], xsT[s_]], writes=[PL[b_]])
            P.op("dve", ts(gg[b_].ap, PG[b_].ap[:, 0:SUB], binb[ws_].ap[:, j:j + 1], 7.0, ALU.add, ALU.min), reads=[PG[b_], binb[ws_]], writes=[gg[b_]])
            P.op("act", actf(sg[b_].ap, gg[b_].ap, AF.Sigmoid, scale=1.702), reads=[gg[b_]], writes=[sg[b_]])
            P.op("dve", ts(ll[b_].ap, PL[b_].ap[:, 0:SUB], binb[ws_].ap[:, 8 + j:9 + j], 7.0, ALU.add, ALU.min), reads=[PL[b_], binb[ws_]], writes=[ll[b_]])
            P.op("dve", ts(ll[b_].ap, ll[b_].ap, -7.0, 1.0, ALU.max, ALU.add), reads=[ll[b_]], writes=[ll[b_]])
            P.op("pool", tt(gg[b_].ap, gg[b_].ap, sg[b_].ap, ALU.mult), reads=[gg[b_], sg[b_]], writes=[gg[b_]])
            P.op("pool", tt(actT.ap[:, j, :], gg[b_].ap, ll[b_].ap, ALU.mult), reads=[gg[b_], ll[b_]], writes=[actT])
        if pi + 1 < len(passes):
            emit_xsT(pi + 1)
        if pi + 2 < len(passes):
            load_xs(pi + 2)
        for sl in range(nslot):
            for h in range(2):
                py = PY[(sl * 2 + h) % 2]
                P.pe_group([mm(py.ap, actT.ap[:, j, sl * 128:(sl + 1) * 128], wout[ws_].ap[:, j, h * 512:(h + 1) * 512], j == 0, j == 7) for j in range(8)],
                           reads=[actT, wout[ws_]], writes=[py])
                P.op("dve", tt(ysst.ap[:, sl, h * 512:(h + 1) * 512], py.ap, boutbc[ws_].ap[:, h * 512:(h + 1) * 512], ALU.add),
                     reads=[py, boutbc[ws_]], writes=[ysst])
        P.dma("sp", dmaf(ys_scr.ap[r0:r0 + SUB, :].rearrange("(s p) d -> p s d", p=128), ysst.ap[:, 0:nslot, :]), "ysst",
              reads=[ysst], writes=[ys_scr])
    P.barrier()
    A.off = mark3

    yg = [[A.alloc([128, 1024], F32, f"yg{i}{k}") for k in range(4)] for i in range(2)]
    x1l = [A.alloc([128, 1024], F32, f"x1l{i}") for i in range(2)]
    acc = [A.alloc([128, 1024], F32, f"acc{i}") for i in range(2)]
    ot = [A.alloc([128, 1024], F32, f"ot{i}") for i in range(2)]
    gfin = A.alloc([128, 1024], F32, "gfin")
    junk2 = A.alloc([128, 1024], BF16, "junk4")
    P.dma("sp", dmaf(gfin.ap, D["g_fin"].partition_broadcast(128)), "gfin", writes=[gfin])
    out_toks = []
    for i in range(16):
        s_ = i % 2
        st_ = sm[s_]
        P.dma("sp", dmaf(x1l[s_].ap, x1_scr.ap[i * 128:(i + 1) * 128, :]), f"x1l{s_}", reads=[x1_scr], writes=[x1l[s_]])
        for k in range(4):
            P.dma("pool", lambda e, i=i, k=k, s_=s_: e.indirect_dma_start(
                out=yg[s_][k].ap, out_offset=None, in_=ys_scr.ap,
                in_offset=bass.IndirectOffsetOnAxis(ap=rowi.ap[:, i, k:k + 1], axis=0)), f"yg{s_}{k}",
                reads=[rowi, ys_scr], writes=[yg[s_][k]])
        P.op("dve", ts(acc[s_].ap, yg[s_][0].ap, gkk.ap[:, i, 0:1], None, ALU.mult), reads=[yg[s_][0], gkk], writes=[acc[s_]])
        for k in range(1, 4):
            P.op("dve", stt(acc[s_].ap, yg[s_][k].ap, gkk.ap[:, i, k:k + 1], acc[s_].ap, ALU.mult, ALU.add),
                 reads=[yg[s_][k], gkk, acc[s_]], writes=[acc[s_]])
        P.op("dve", tt(acc[s_].ap, acc[s_].ap, modbc[7].ap, ALU.mult), reads=[acc[s_], modbc[7]], writes=[acc[s_]])
        P.op("dve", tt(acc[s_].ap, acc[s_].ap, x1l[s_].ap, ALU.add), reads=[acc[s_], x1l[s_]], writes=[acc[s_]])
        P.op("act", actf(junk2.ap, acc[s_].ap, AF.Square, accum=st_.ap[:, 0:1]), reads=[acc[s_]], writes=[junk2, st_])
        P.op("act", actf(st_.ap[:, 1:2], st_.ap[:, 0:1], AF.Ln, bias=EPS, scale=1.0 / 1024.0), reads=[st_], writes=[st_])
        P.op("act", actf(st_.ap[:, 2:3], st_.ap[:, 1:2], AF.Exp, scale=-0.5), reads=[st_], writes=[st_])
        P.op("dve", stt(ot[s_].ap, acc[s_].ap, st_.ap[:, 2:3], gfin.ap, ALU.mult, ALU.mult), reads=[acc[s_], st_, gfin], writes=[ot[s_]])
        out_toks.append(P.dma("sp", dmaf(out_d[i * 128:(i + 1) * 128, :], ot[s_].ap), f"ot{s_}", reads=[ot[s_]]))
    P.wait_all("sp", out_toks)


def _partner_perm():
    j = np.arange(64)
    a, h, f = j // 32, (j % 32) // 16, j % 16
    return a * 32 + (1 - h) * 16 + f


_NC_CACHE = {}


def prepare_inputs(x, c, ctx, c_ctx, w_ada, b_ada, g_attn, w_qkv, gqa_q_norm, gqa_k_norm, diff_lambda,
                   diff_subln, w_o, g_ffn, w_router, b_router, w_in, b_in, w_out, b_out, g_final):
    f = lambda a: np.ascontiguousarray(np.asarray(a, dtype=np.float32))
    x, c, ctx, c_ctx = f(x), f(c), f(ctx), f(c_ctx)
    wqkv = f(w_qkv)[0]
    part = _partner_perm()

    def rot_cols(w, nheads):
        idx = (np.arange(nheads)[:, None] * 64 + part[None, :]).reshape(-1)
        return w[:, idx]

    qa, ka, va = wqkv[:, 0:512], wqkv[:, 512:640], wqkv[:, 640:768]
    qb, kb, vb = wqkv[:, 768:1280], wqkv[:, 1280:1792], wqkv[:, 1792:2304]
    wkv = np.ascontiguousarray(np.concatenate([ka, kb, rot_cols(ka, 2), rot_cols(kb, 8), va, vb], axis=1))
    wq = np.ascontiguousarray(np.concatenate([qa, qb, rot_cols(qa, 8), rot_cols(qb, 8)], axis=1))
    p = np.arange(128)
    j = p % 64
    gqn, gkn = f(gqa_q_norm)[0], f(gqa_k_norm)[0]
    gq = np.ascontiguousarray(np.stack([gqn[j], gqn[part[j]]], axis=1))
    gk = np.ascontiguousarray(np.stack([gkn[j], gkn[part[j]]], axis=1))
    pmeta = np.zeros((128, 4), np.float32)
    pmeta[:, 0] = j % 16
    pmeta[:, 1] = j // 32
    pmeta[:, 2] = np.where((j % 32) // 16 == 0, -1.0, 1.0)
    b_in_l = np.ascontiguousarray(f(b_in)[0].reshape(NE, 16, 128).transpose(0, 2, 1))
    shared = dict(pmeta=pmeta, w_ada=f(w_ada)[0], b_ada=f(b_ada)[0], g_attn=f(g_attn)[0], wkv=wkv, wq=wq, gq=gq, gk=gk,
                  dlam=f(diff_lambda)[0].reshape(256), subln=f(diff_subln)[0], w_o=f(w_o)[0], g_ffn=f(g_ffn)[0],
                  w_r=f(w_router)[0], b_r=f(b_router)[0], w_in=f(w_in)[0], b_in=b_in_l, w_out=f(w_out)[0],
                  b_out=f(b_out)[0], g_fin=f(g_final))
    in_maps = []
    for core in range(8):
        b, qi = core // 4, core % 4
        m = dict(shared)
        m["xkv"] = np.ascontiguousarray(np.concatenate([ctx[b], x[b]], axis=0))
        m["xq"] = np.ascontiguousarray(x[b, qi * NQ:(qi + 1) * NQ])
        cv = np.stack([c[b], c_ctx], axis=1)
        m["cvec"] = np.ascontiguousarray(cv.reshape(8, 128, 2).transpose(1, 0, 2).reshape(128, 16))
        m["pos0"] = np.full((128, 1), float(qi * NQ // 64), np.float32)
        in_maps.append(m)
    return in_maps


def kernel(**inputs):
    in_maps = prepare_inputs(**inputs)
    if "nc" not in _NC_CACHE:
        _NC_CACHE["nc"] = build_nc()
    res = run_bass_kernel_spmd(_NC_CACHE["nc"], in_maps, core_ids=list(range(8)))
    out = np.zeros((2, 8192, 1024), np.float32)
    for core in range(8):
        b, qi = core // 4, core % 4
        out[b, qi * NQ:(qi + 1) * NQ] = res.results[core]["out"]
    return out
```
